# Optimizing a Trainium2 kernel written in Bass

```python
import jax, jax.numpy as jnp
from jax import lax
import numpy as np

D_MODEL = 2048
BATCH = 4
SEQ = 2048
DEPTH = 1

HEAD_DIM = 64
N_ATTN_HEADS = 16
ATTN_WIDTH = N_ATTN_HEADS * HEAD_DIM
LRU_WIDTH = D_MODEL - ATTN_WIDTH
N_LRU_BLOCKS = 16
LRU_BLOCK = LRU_WIDTH // N_LRU_BLOCKS
CONV_WIDTH = 4
LRU_C = 8.0
IN_COLS = 3 * ATTN_WIDTH + 2 * LRU_WIDTH
DILATED_BRANCHES = ((128, 1), (512, 4), (2048, 16))
ATTN_BLOCK = 128
N_EXPERTS = 32
TOP_K = 4
D_FF = D_MODEL
SWIGLU_LIMIT = 7.0
SWIGLU_ALPHA = 1.702
MOE_BLOCK = 128
EPS = 1e-6

kernel_name = 'hybrid_dilated_attn_rglru_moe_layer'


def rms_norm(x, g):
    xf = x.astype(jnp.float32)
    y = xf * lax.rsqrt(jnp.mean(xf * xf, axis=-1, keepdims=True) + EPS)
    return (y * g.astype(jnp.float32)).astype(x.dtype)


def alibi_slopes(n_heads):
    return jnp.asarray(2.0 ** (-8.0 * np.arange(1, n_heads + 1) / n_heads), jnp.float32)


def dilated_branch(q, k, v, slopes, window, dilation):
    B, S, H, Dh = q.shape
    n_back = window // dilation
    assert n_back <= ATTN_BLOCK
    span = dilation * ATTN_BLOCK
    S_pad = -(-S // span) * span
    L = S_pad // dilation
    nb = L // ATTN_BLOCK

    def to_blocks(t):
        t = jnp.pad(t, ((0, 0), (0, S_pad - S), (0, 0), (0, 0)))
        t = t.reshape(B, L, dilation, H, Dh).transpose(0, 2, 3, 1, 4)
        return t.reshape(B, dilation, H, nb, ATTN_BLOCK, Dh)

    def with_prev(t):
        prev = jnp.pad(t[:, :, :, :-1], ((0, 0), (0, 0), (0, 0), (1, 0), (0, 0), (0, 0)))
        return jnp.concatenate([prev, t], axis=4)

    qb = to_blocks(q)
    kk = with_prev(to_blocks(k))
    vv = with_prev(to_blocks(v))
    scores = jnp.einsum('bdhnqe,bdhnke->bdhnqk', qb, kk).astype(jnp.float32) * (Dh ** -0.5)
    qi = jnp.arange(ATTN_BLOCK)[:, None] + ATTN_BLOCK
    ki = jnp.arange(2 * ATTN_BLOCK)[None, :]
    rel = qi - ki
    key_pos = jnp.arange(nb)[:, None, None] * ATTN_BLOCK + ki[None] - ATTN_BLOCK
    mask = (rel >= 0)[None] & (rel <= n_back)[None] & (key_pos >= 0)
    bias = -slopes[:, None, None] * (rel * dilation).astype(jnp.float32)[None]
    scores = jnp.where(mask[None, None, None], scores + bias[None, None, :, None], -jnp.inf)
    m = jnp.max(scores, axis=-1, keepdims=True)
    p = jnp.exp(scores - m)
    s = jnp.sum(p, axis=-1, keepdims=True)
    o = jnp.einsum('bdhnqk,bdhnke->bdhnqe', p, vv.astype(jnp.float32)) / s
    lse = (m + jnp.log(s))[..., 0]
    o = o.reshape(B, dilation, H, L, Dh).transpose(0, 3, 1, 2, 4).reshape(B, S_pad, H, Dh)[:, :S]
    lse = lse.reshape(B, dilation, H, L).transpose(0, 3, 1, 2).reshape(B, S_pad, H)[:, :S]
    return o, lse


def dilated_mixture_attention(q, k, v):
    slopes = alibi_slopes(q.shape[2])
    outs, lses = [], []
    for window, dilation in DILATED_BRANCHES:
        o, lse = dilated_branch(q, k, v, slopes, window, dilation)
        outs.append(o)
        lses.append(lse)
    w = jax.nn.softmax(jnp.stack(lses, axis=0), axis=0)
    o = jnp.einsum('nbsh,nbshe->bshe', w, jnp.stack(outs, axis=0))
    return o.astype(q.dtype)


def causal_depthwise_conv(x, w, b):
    S = x.shape[1]
    xp = jnp.pad(x, ((0, 0), (CONV_WIDTH - 1, 0), (0, 0)))
    out = xp[:, 0:S] * w[0]
    for i in range(1, CONV_WIDTH):
        out = out + xp[:, i:i + S] * w[i]
    return out + b


def block_diag_linear(x, w, b):
    B, S, _ = x.shape
    xb = x.reshape(B, S, N_LRU_BLOCKS, LRU_BLOCK)
    y = jnp.einsum('bsnc,ncd->bsnd', xb, w.astype(jnp.float32)).reshape(B, S, LRU_WIDTH)
    return y + b.astype(jnp.float32)


def rg_lru(x, w_a, b_a, w_x, b_x, lam):
    xf = x.astype(jnp.float32)
    r = jax.nn.sigmoid(block_diag_linear(xf, w_a, b_a))
    i = jax.nn.sigmoid(block_diag_linear(xf, w_x, b_x))
    log_a = -LRU_C * r * jax.nn.softplus(-lam.astype(jnp.float32))
    a = jnp.exp(log_a)
    b = jnp.sqrt(-jnp.expm1(2.0 * log_a)) * (i * xf)

    def combine(c1, c2):
        a1, b1 = c1
        a2, b2 = c2
        return a1 * a2, a2 * b1 + b2

    _, h = lax.associative_scan(combine, (a, b), axis=1)
    return h.astype(x.dtype)


def moe_ffn(h, w_router, b_router, w_gate_up, b_gate_up, w_down, b_down):
    B, S, D = h.shape
    N = B * S
    NK = N * TOP_K
    xt = h.reshape(N, D)
    logits = xt.astype(jnp.float32) @ w_router.astype(jnp.float32) + b_router.astype(jnp.float32)
    top_val, top_idx = lax.top_k(logits, TOP_K)
    gates = jax.nn.softmax(top_val, axis=-1)
    e_flat = top_idx.reshape(NK)
    tok_flat = jnp.repeat(jnp.arange(N, dtype=jnp.int32), TOP_K)
    order = jnp.argsort(e_flat)
    e_sorted = e_flat[order]
    counts = jnp.bincount(e_flat, length=N_EXPERTS)
    start = jnp.cumsum(counts) - counts
    padded = (counts + MOE_BLOCK - 1) // MOE_BLOCK * MOE_BLOCK
    pend = jnp.cumsum(padded)
    pstart = pend - padded
    dest_sorted = (pstart[e_sorted] + jnp.arange(NK) - start[e_sorted]).astype(jnp.int32)
    P = NK + N_EXPERTS * MOE_BLOCK
    n_blocks = P // MOE_BLOCK
    tok_buf = jnp.full((P,), N, jnp.int32).at[dest_sorted].set(tok_flat[order])
    block_expert = jnp.minimum(
        jnp.searchsorted(pend, jnp.arange(n_blocks) * MOE_BLOCK, side='right'), N_EXPERTS - 1)
    x_pad = jnp.concatenate([xt, jnp.zeros((1, D), xt.dtype)], axis=0)

    def run_block(args):
        tok, e = args
        xb = x_pad[tok]
        gu = xb @ w_gate_up[e] + b_gate_up[e]
        gate = jnp.minimum(gu[:, 0::2], SWIGLU_LIMIT)
        up = jnp.clip(gu[:, 1::2], -SWIGLU_LIMIT, SWIGLU_LIMIT)
        act = gate * jax.nn.sigmoid(SWIGLU_ALPHA * gate) * (up + 1.0)
        return act @ w_down[e] + b_down[e]

    y_buf = lax.map(run_block, (tok_buf.reshape(n_blocks, MOE_BLOCK), block_expert))
    y_buf = y_buf.reshape(P, D)
    dest = jnp.zeros((NK,), jnp.int32).at[order].set(dest_sorted)
    y = y_buf[dest].reshape(N, TOP_K, D)
    out = jnp.einsum('nk,nkd->nd', gates.astype(y.dtype), y)
    return out.reshape(B, S, D)


def setup_inputs(seed: int = 0) -> dict:
    key = jax.random.key(seed)
    ks = jax.random.split(key, 22)
    f32 = jnp.float32

    def nrm(k, shape, scale):
        return jax.random.normal(k, shape, f32) * scale

    def gain(k, n):
        return 1.0 + 0.02 * jax.random.normal(k, (n,), f32)

    a_c = jax.random.uniform(ks[9], (LRU_WIDTH,), f32, 0.9, 0.999)
    a0 = a_c ** (1.0 / LRU_C)
    lru_lambda = jnp.log(a0) - jnp.log1p(-a0)
    return {
        'x': jax.random.normal(ks[0], (BATCH, SEQ, D_MODEL), f32),
        'norm_mix': gain(ks[1], D_MODEL),
        'w_in': nrm(ks[2], (D_MODEL, IN_COLS), D_MODEL ** -0.5),
        'conv_w': nrm(ks[3], (CONV_WIDTH, LRU_WIDTH), CONV_WIDTH ** -0.5),
        'conv_b': nrm(ks[4], (LRU_WIDTH,), 0.02),
        'w_a': nrm(ks[5], (N_LRU_BLOCKS, LRU_BLOCK, LRU_BLOCK), LRU_BLOCK ** -0.5),
        'b_a': nrm(ks[6], (LRU_WIDTH,), 0.02),
        'w_x': nrm(ks[7], (N_LRU_BLOCKS, LRU_BLOCK, LRU_BLOCK), LRU_BLOCK ** -0.5),
        'b_x': nrm(ks[8], (LRU_WIDTH,), 0.02),
        'lru_lambda': lru_lambda,
        'attn_out_norm': gain(ks[10], ATTN_WIDTH),
        'lru_out_norm': gain(ks[11], LRU_WIDTH),
        'w_out': nrm(ks[12], (D_MODEL, D_MODEL), D_MODEL ** -0.5),
        'norm_ffn': gain(ks[13], D_MODEL),
        'w_router': nrm(ks[14], (D_MODEL, N_EXPERTS), D_MODEL ** -0.5),
        'b_router': nrm(ks[15], (N_EXPERTS,), 0.01),
        'w_gate_up': nrm(ks[16], (N_EXPERTS, D_MODEL, 2 * D_FF), D_MODEL ** -0.5),
        'b_gate_up': nrm(ks[17], (N_EXPERTS, 2 * D_FF), 0.02),
        'w_down': nrm(ks[18], (N_EXPERTS, D_FF, D_MODEL), D_FF ** -0.5),
        'b_down': nrm(ks[19], (N_EXPERTS, D_MODEL), 0.02),
        'norm_final': gain(ks[20], D_MODEL),
    }


def reference(x, norm_mix, w_in, conv_w, conv_b, w_a, b_a, w_x, b_x, lru_lambda,
              attn_out_norm, lru_out_norm, w_out, norm_ffn, w_router, b_router,
              w_gate_up, b_gate_up, w_down, b_down, norm_final):
    B, S, _ = x.shape
    for _layer in range(DEPTH):
        h = rms_norm(x, norm_mix)
        proj = h @ w_in
        q, k, v, xr, gr = jnp.split(
            proj, [ATTN_WIDTH, 2 * ATTN_WIDTH, 3 * ATTN_WIDTH, 3 * ATTN_WIDTH + LRU_WIDTH], axis=-1)
        q = q.reshape(B, S, N_ATTN_HEADS, HEAD_DIM)
        k = k.reshape(B, S, N_ATTN_HEADS, HEAD_DIM)
        v = v.reshape(B, S, N_ATTN_HEADS, HEAD_DIM)
        attn = dilated_mixture_attention(q, k, v).reshape(B, S, ATTN_WIDTH)
        xr = causal_depthwise_conv(xr, conv_w, conv_b)
        rec = rg_lru(xr, w_a, b_a, w_x, b_x, lru_lambda) * jax.nn.gelu(gr)
        mixed = jnp.concatenate([rms_norm(attn, attn_out_norm), rms_norm(rec, lru_out_norm)], axis=-1)
        x = x + mixed @ w_out
        x = x + moe_ffn(rms_norm(x, norm_ffn), w_router, b_router, w_gate_up, b_gate_up, w_down, b_down)
    return rms_norm(x, norm_final)
```

```python
import contextlib
import numpy as np
import concourse.bass as bass
import concourse.mybir as mybir
from concourse.alu_op_type import AluOpType as ALU
from concourse.bass_utils import run_bass_kernel_spmd

F32 = mybir.dt.float32
BF16 = mybir.dt.bfloat16
AF = mybir.ActivationFunctionType

D = 2048
WIN = 2048
OWN = 1024
NDC = 16
NE = 32
CAP = 256
EPS = 1e-6
NRING = 3
KPRE = 9
ENGS = ("sync", "act", "pool", "dve", "pe")


def ssl(start, n, step):
    return slice(start, start + (n - 1) * step + 1, step)


class Buf:
    __slots__ = ("name", "lw", "rd", "excl")

    def __init__(self, name="", excl=False):
        self.name = name
        self.lw = None
        self.rd = {}
        self.excl = excl


class DSem:
    def __init__(self, h, key):
        self.h = h
        self.key = key
        self.count = 0


class Sched:
    SELF_SYNC = ("act", "pool", "dve")

    def __init__(self, nc, stack):
        self.nc = nc
        self.stack = stack
        self.items = {e: [] for e in ENGS}
        self.cnt = {e: 0 for e in ENGS}
        self.waited = {e: {} for e in ENGS}
        self.semh = {}
        for e in ENGS:
            self.semh[("eng", e)] = stack.enter_context(nc.semaphore("s_" + e))
        self.ndsem = 0
        self.nops = 0

    def dsem(self):
        k = ("dma", self.ndsem)
        self.semh[k] = self.stack.enter_context(self.nc.semaphore(f"d{self.ndsem}"))
        self.ndsem += 1
        return DSem(self.semh[k], k)

    def _collect(self, eng, reads, writes):
        deps = {}

        def add(k, v):
            if deps.get(k, 0) < v:
                deps[k] = v
        for b in reads:
            if b.lw is not None:
                add(*b.lw)
        for b in writes:
            if b.lw is not None:
                add(*b.lw)
            for k, v in b.rd.items():
                add(k, v)
        out = []
        for k, v in deps.items():
            if k == ("eng", eng) and eng not in self.SELF_SYNC:
                continue
            if self.waited[eng].get(k, 0) >= v:
                continue
            self.waited[eng][k] = v
            out.append((k, v))
        return out

    def _mark(self, ev, reads, writes):
        k, v = ev
        for b in reads:
            if b.rd.get(k, 0) < v:
                b.rd[k] = v
        for b in writes:
            b.lw = ev
            b.rd = {}

    def op(self, eng, fn, reads=(), writes=()):
        ex = [b for b in reads if b.excl]
        if ex:
            writes = list(writes) + [b for b in ex if b not in writes]
            reads = [b for b in reads if not b.excl]
        waits = self._collect(eng, reads, writes)
        self.cnt[eng] += 1
        ev = (("eng", eng), self.cnt[eng])
        self.items[eng].append((waits, fn, ev, 1))
        self._mark(ev, reads, writes)
        self.nops += 1
        return ev

    def dma(self, q, out, in_, ds, reads=(), writes=(), **kw):
        waits = self._collect(q, reads, writes)
        ds.count += 16
        ev = (ds.key, ds.count)
        self.items[q].append(
            (waits, (lambda e, out=out, in_=in_, kw=kw: e.dma_start(out=out, in_=in_, **kw)), ev, 16))
        self._mark(ev, reads, writes)
        self.nops += 1
        return ev

    def alias(self, olds, new):
        for b in olds:
            if b.lw is not None:
                k, v = b.lw
                if new.rd.get(k, 0) < v:
                    new.rd[k] = v
            for k, v in b.rd.items():
                if new.rd.get(k, 0) < v:
                    new.rd[k] = v

    def final_wait(self, eng, evs):
        waits = []
        for k, v in evs:
            if self.waited[eng].get(k, 0) < v:
                self.waited[eng][k] = v
                waits.append((k, v))
        self.items[eng].append((waits, None, None, 0))

    def emit(self):
        nc = self.nc
        engobj = {"sync": "sync", "act": "scalar", "pool": "gpsimd", "dve": "vector", "pe": "tensor"}
        semh = self.semh
        with nc.Block() as block:
            for e in ENGS:
                def body(eng, items=self.items[e]):
                    for waits, fn, ev, inc in items:
                        for k, v in waits:
                            eng.wait_ge(semh[k], v)
                        if fn is not None:
                            fn(eng).then_inc(semh[ev[0]], inc)
                getattr(block, engobj[e])(body)


CST_IDENT = 0
CST_R2 = 128
CST_R16 = 640
CST_IOTA = 1664
CST_LTRI = 1920
NCST = 2048
MASKV = -1.0e6


def _make_cst():
    c = np.zeros((128, NCST), np.float32)
    c[:, CST_IDENT:CST_IDENT + 128] = np.eye(128, dtype=np.float32)
    k = np.arange(128)[:, None].astype(np.float64)
    q = np.arange(128)[None, :].astype(np.float64)
    rprev = np.where(q <= k, -(q + 128 - k), MASKV)
    rcur = np.where(q >= k, -(q - k), MASKV)
    r2 = np.concatenate([rprev, rcur, rprev, rcur], axis=1)
    c[:, CST_R2:CST_R2 + 512] = r2
    for g in range(2):
        blk = rcur[:, 64 + 32 * g: 64 + 32 * g + 32]
        c[:, CST_R16 + g * 512: CST_R16 + (g + 1) * 512] = np.tile(blk, (1, 16))
    c[:, CST_IOTA:CST_IOTA + 256] = np.arange(256, dtype=np.float32)[None, :]
    c[:, CST_LTRI:CST_LTRI + 128] = (k < q).astype(np.float32)
    return c


SM_GMIXT = 0
SM_CONVW = 16
SM_CONVB = 48
SM_BA = 56
SM_BX = 64
SM_LAM = 72
SM_BROUT = 80
SM_FLAG = 112
NSM = 128


def build(debug=None):
    nc = bass.Bass("TRN2", target_bir_lowering=False)

    def din(name, shape, dt=F32):
        return nc.dram_tensor(name, list(shape), dt, kind="ExternalInput").ap()

    xw = din("xw", [WIN, D])
    cst_d = din("cst", [128, NCST])
    sm_d = din("smalls", [128, NSM])
    gmix_d = din("gmix_bc", [128, D])
    gffn_d = din("gffn_bc", [128, D])
    gfin_d = din("gfin_bc", [128, D])
    watt_d = din("w_att", [8, 128, NDC, 384])
    wlru_d = din("w_lru", [8, 128, NDC, 256])
    wabd_d = din("wabd", [8, 128, 256])
    wout_d = din("w_out", [D, D])
    wrt_d = din("w_router_t", [128, NDC, NE])
    bgu_d = din("bgu", [128, NE * 32])
    bdn_d = din("b_down", [NE, D])
    big = debug in (None, "moe1", "x3")
    wgu_d = din("wgu_t", [NE, 8, 128, 8192]) if big else None
    wdn_d = din("wdn_t", [NE, 4, 128, 8192]) if big else None
    out_d = nc.dram_tensor("out", [OWN, D], F32, kind="ExternalOutput").ap()
    wgu_bf = nc.dram_tensor("wgu_bf", [NE, 3, 128, 8192], BF16, kind="Internal").ap() if big else None
    watt_bf = nc.dram_tensor("watt_bf", [8, 128, NDC * 384], BF16, kind="Internal").ap()
    wlru_bf = nc.dram_tensor("wlru_bf", [8, 128, NDC * 256], BF16, kind="Internal").ap()
    wo_bf = nc.dram_tensor("wo_bf", [4, 128, 8192], BF16, kind="Internal").ap()
    dbg_d = None
    if debug is not None:
        dbg_d = nc.dram_tensor("dbg", [128, 16384], F32, kind="ExternalOutput").ap()

    with contextlib.ExitStack() as st:
        S = Sched(nc, st)
        ARENA_KB = 207
        arena = st.enter_context(nc.sbuf_tensor("arena", [128, ARENA_KB * 256], F32))

        def V(off_kb, nbytes, dt=F32, pat=None, **kw):
            off = int(round(off_kb * 1024))
            assert off % 4 == 0 and nbytes % 4 == 0 and off + nbytes <= ARENA_KB * 1024, (off_kb, nbytes)
            a = arena[:, off // 4:(off + nbytes) // 4]
            if dt != F32:
                a = a.bitcast(dt)
            if pat:
                a = a.rearrange(pat, **kw)
            return a

        banks = [st.enter_context(nc.psum_tensor(f"pb{i}", [128, 512], F32)) for i in range(8)]
        PB = [Buf(f"pb{i}", excl=True) for i in range(8)]

        def bank_bf(i):
            return banks[i][:].bitcast(BF16)

        K_CST = 189
        cst = V(K_CST, NCST * 4)
        sm = V(K_CST + 8, 1024)
        identB = V(K_CST + 9, 256, BF16)
        ltriB = V(K_CST + 9.25, 256, BF16)
        onesB = V(K_CST + 9.5, 256, BF16)
        flag64 = V(K_CST + 9.75, 256, BF16)
        flag16_64 = V(K_CST + 10, 256, BF16)
        ones64 = onesB
        onesF = V(K_CST + 10.25, 512)
        bgu = V(K_CST + 11, 4096)
        small_t = V(K_CST + 15, 2048)
        CST, SM, BGU, STAT = Buf("cst"), Buf("sm"), Buf("bgu"), Buf("stat")
        identF = cst[:, CST_IDENT:CST_IDENT + 128]
        R2 = cst[:, CST_R2:CST_R2 + 512]
        R16 = [cst[:, CST_R16 + g * 512:CST_R16 + (g + 1) * 512] for g in range(2)]
        iota = cst[:, CST_IOTA:CST_IOTA + 256]
        flag = sm[:, SM_FLAG:SM_FLAG + 1]
        flag16 = sm[:, SM_FLAG + 1:SM_FLAG + 2]
        SMD = 128
        c_lru = sm[:, SMD:SMD + 8]
        c2_lru = sm[:, SMD + 8:SMD + 16]
        tmpA = sm[:, SMD + 16:SMD + 24]
        tmpB = sm[:, SMD + 24:SMD + 32]
        tmpC = sm[:, SMD + 32:SMD + 40]
        s0t = sm[:, SMD + 40:SMD + 41]

        dcst = S.dsem()
        S.dma("sync", cst, cst_d, dcst, writes=[CST])
        S.dma("sync", sm[:, 0:NSM], sm_d, dcst, writes=[SM])
        S.dma("sync", bgu, bgu_d, dcst, writes=[BGU])
        for b in (CST, SM, BGU):
            b.lw = (dcst.key, dcst.count)

        CB = Buf("constsB")
        S.op("dve", lambda e: e.tensor_copy(out=identB, in_=identF), reads=[CST], writes=[CB])
        S.op("dve", lambda e: e.tensor_copy(out=ltriB, in_=cst[:, CST_LTRI:CST_LTRI + 128]), reads=[CST], writes=[CB])
        S.op("dve", lambda e: e.memset(onesB, 1.0), writes=[CB])
        S.op("dve", lambda e: e.memset(onesF, 1.0), writes=[CB])
        S.op("dve", lambda e: e.tensor_scalar(out=flag64, in0=ones64, scalar1=flag, scalar2=None, op0=ALU.mult),
             reads=[SM, CB], writes=[CB])
        S.op("dve", lambda e: e.tensor_scalar(out=flag16_64, in0=ones64, scalar1=flag16, scalar2=None, op0=ALU.mult),
             reads=[SM, CB], writes=[CB])
        bgu3 = bgu.rearrange("p (e j) -> p e j", j=32)
        S.op("dve", lambda e: e.tensor_scalar(out=bgu3[:, :, 16:32], in0=bgu3[:, :, 16:32], scalar1=1.0, scalar2=None,
                                              op0=ALU.add), reads=[BGU], writes=[BGU])
        lam = sm[:, SM_LAM:SM_LAM + 8]
        S.op("act", lambda e: e.activation(out=tmpA, in_=lam, func=AF.Exp, scale=-1.0), reads=[SM], writes=[SM])
        S.op("dve", lambda e: e.tensor_scalar(out=tmpB, in0=tmpA, scalar1=2.0, scalar2=None, op0=ALU.add), reads=[SM], writes=[SM])
        S.op("dve", lambda e: e.reciprocal(out=tmpB, in_=tmpB), reads=[SM], writes=[SM])
        S.op("dve", lambda e: e.tensor_tensor(out=tmpA, in0=tmpA, in1=tmpB, op=ALU.mult), reads=[SM], writes=[SM])
        S.op("dve", lambda e: e.tensor_tensor(out=tmpB, in0=tmpA, in1=tmpA, op=ALU.mult), reads=[SM], writes=[SM])
        S.op("dve", lambda e: e.tensor_scalar(out=tmpC, in0=tmpB, scalar1=1.0 / 7, scalar2=1.0 / 5, op0=ALU.mult, op1=ALU.add), reads=[SM], writes=[SM])
        S.op("dve", lambda e: e.tensor_tensor(out=tmpC, in0=tmpC, in1=tmpB, op=ALU.mult), reads=[SM], writes=[SM])
        S.op("dve", lambda e: e.tensor_scalar(out=tmpC, in0=tmpC, scalar1=1.0 / 3, scalar2=None, op0=ALU.add), reads=[SM], writes=[SM])
        S.op("dve", lambda e: e.tensor_tensor(out=tmpC, in0=tmpC, in1=tmpB, op=ALU.mult), reads=[SM], writes=[SM])
        S.op("dve", lambda e: e.tensor_scalar(out=tmpC, in0=tmpC, scalar1=1.0, scalar2=None, op0=ALU.add), reads=[SM], writes=[SM])
        S.op("dve", lambda e: e.tensor_tensor(out=tmpC, in0=tmpC, in1=tmpA, op=ALU.mult), reads=[SM], writes=[SM])
        S.op("dve", lambda e: e.tensor_scalar(out=c_lru, in0=tmpC, scalar1=-16.0, scalar2=None, op0=ALU.mult), reads=[SM], writes=[SM])
        S.op("dve", lambda e: e.tensor_scalar(out=c2_lru, in0=tmpC, scalar1=-32.0, scalar2=None, op0=ALU.mult), reads=[SM], writes=[SM])

        hT = V(0, 65536, BF16, "p (c t) -> p c t", c=NDC)
        HT = [Buf(f"hT{t}") for t in range(16)]
        mixedT = V(64, 32768, BF16, "p (c t) -> p c t", c=16)
        MX = [Buf(f"mx{c}") for c in range(16)]
        wring = [V(96 + 12 * i, 12288, BF16, "p (c n) -> p c n", c=NDC) for i in range(2)]
        WR = [Buf("wr0"), Buf("wr1")]
        dwr = [S.dsem(), S.dsem()]
        TK = 120
        gmix_bc = V(168, 8192)
        GM = Buf("gmix")
        dg = S.dsem()
        S.dma("sync", gmix_bc, gmix_d, dg, writes=[GM])

        units = [("att", i) for i in range(8)] + [("lru", i) for i in range(8)]

        NPS = 4
        dpre = [S.dsem() for _ in range(NPS)]
        pre_list = [("unit", 0, ui) for ui in range(2, 16)] + [("wo", 0, n4) for n4 in range(4)]
        if big:
            for ex in range(NE):
                for pc in (1, 4, 7):
                    pre_list.append(("gu", ex, pc))
        PCB = {}
        pre_evs = []
        pre_i = [0]

        def issue_precast(n):
            for _ in range(n):
                i = pre_i[0]
                if i >= len(pre_list):
                    return
                pre_i[0] += 1
                kind, ex, j = pre_list[i]
                if kind == "wo":
                    src = wout_d.rearrange("(c p) n -> p c n", p=128)[:, :, j * 512:(j + 1) * 512]
                    dst = wo_bf[j].rearrange("p (c n) -> p c n", c=NDC)
                elif kind == "unit":
                    ukind, ui_ = units[j]
                    if ukind == "att":
                        src = watt_d[ui_]
                        dst = watt_bf[ui_].rearrange("p (c n) -> p c n", c=NDC)
                    else:
                        src = wlru_d[ui_]
                        dst = wlru_bf[ui_].rearrange("p (c n) -> p c n", c=NDC)
                else:
                    src = wgu_d[ex, j].rearrange("p (a b) -> p a b", b=2048)
                    dst = wgu_bf[ex, j // 3].rearrange("p (a b) -> p a b", b=2048)
                bb = Buf(f"pre{i}")
                PCB[(kind, ex, j)] = bb
                if i >= NPS:
                    bb.rd[pre_evs[i - NPS][0]] = pre_evs[i - NPS][1]
                ev = S.dma("pool", dst, src, dpre[i % NPS], writes=[bb])
                pre_evs.append(ev)

        def issue_unit_dma(ui):
            kind, i = units[ui]
            s = ui % 2
            if ui < 2:
                if kind == "att":
                    S.dma("pool", wring[s][:, :, 0:384], watt_d[i], dwr[s], writes=[WR[s]])
                else:
                    S.dma("pool", wring[s][:, :, 0:256], wlru_d[i], dwr[s], writes=[WR[s]])
                if ui == 1:
                    issue_precast(100000)
            else:
                pb_ = PCB[("unit", 0, ui)]
                if kind == "att":
                    S.dma("sync", wring[s][:, :, 0:384], watt_bf[i].rearrange("p (c n) -> p c n", c=NDC), dwr[s], reads=[pb_], writes=[WR[s]])
                else:
                    S.dma("sync", wring[s][:, :, 0:256], wlru_bf[i].rearrange("p (c n) -> p c n", c=NDC), dwr[s], reads=[pb_], writes=[WR[s]])

        issue_unit_dma(0)

        xt = [V(TK + 8 * i, 8192) for i in range(2)]
        XT = [Buf("xt0"), Buf("xt1")]
        hb = [V(TK + 16 + 4 * i, 4096, BF16) for i in range(2)]
        HB = [Buf("hb0"), Buf("hb1")]
        sqj = V(TK + 24, 4096, BF16)
        SQJ = Buf("sqj")
        dx = [S.dsem(), S.dsem()]
        ssq = small_t[:, 0:16]
        rs = small_t[:, 16:32]
        pbi = 0
        for tc in range(16):
            s = tc % 2
            S.dma("sync", xt[s], xw[tc * 128:(tc + 1) * 128, :], dx[s], writes=[XT[s]])
            S.op("act", lambda e, s=s, tc=tc: e.activation(out=sqj, in_=xt[s], func=AF.Square, accum_out=ssq[:, tc:tc + 1]),
                 reads=[XT[s]], writes=[SQJ, STAT])
            S.op("dve", lambda e, tc=tc: e.tensor_scalar(out=rs[:, tc:tc + 1], in0=ssq[:, tc:tc + 1], scalar1=1.0 / D, scalar2=EPS,
                                                         op0=ALU.mult, op1=ALU.add), reads=[STAT], writes=[STAT])
            S.op("act", lambda e, tc=tc: e.activation(out=rs[:, tc:tc + 1], in_=rs[:, tc:tc + 1], func=AF.Sqrt), reads=[STAT], writes=[STAT])
            S.op("dve", lambda e, tc=tc: e.reciprocal(out=rs[:, tc:tc + 1], in_=rs[:, tc:tc + 1]), reads=[STAT], writes=[STAT])
            S.op("dve", lambda e, s=s, tc=tc: e.scalar_tensor_tensor(out=hb[s], in0=xt[s], scalar=rs[:, tc:tc + 1], in1=gmix_bc,
                                                                     op0=ALU.mult, op1=ALU.mult),
                 reads=[XT[s], STAT, GM], writes=[HB[s]])
            for q4 in range(4):
                k = pbi % 4
                pbi += 1
                pv = bank_bf(k)[:, 0:512].rearrange("p (j t) -> p j t", j=4)
                for j in range(4):
                    dc = q4 * 4 + j
                    S.op("pe", lambda e, pv=pv, j=j, s=s, dc=dc: e.transpose(out=pv[:, j, :], in_=hb[s][:, dc * 128:(dc + 1) * 128],
                                                                            identity=identB),
                         reads=[HB[s], CB], writes=[PB[k]])
                dst = hT[:, q4 * 4:q4 * 4 + 4, tc * 128:(tc + 1) * 128]
                if q4 % 2 == 0:
                    S.op("act", lambda e, dst=dst, pv=pv: e.activation(out=dst, in_=pv, func=AF.Copy), reads=[PB[k]], writes=[HT[tc]])
                else:
                    S.op("dve", lambda e, dst=dst, pv=pv: e.tensor_copy(out=dst, in_=pv), reads=[PB[k]], writes=[HT[tc]])

        if debug == "A":
            tmpf = V(TK + 28, 16384)
            TMPF = Buf("tmpf")
            S.op("dve", lambda e: e.tensor_copy(out=tmpf, in_=hT[:, 0:2, :].rearrange("p c t -> p (c t)")), reads=HT, writes=[TMPF])
            dd = S.dsem()
            ev = S.dma("sync", dbg_d[:, 0:4096], tmpf, dd, reads=[TMPF])
            S.final_wait("sync", [ev])
            S.emit()
            return nc
        qz = [[V(TK + 0 + 4 * bs + 2 * hh, 2048, BF16) for hh in range(2)] for bs in range(2)]
        QZ = [[Buf(f"qz{bs}{hh}") for hh in range(2)] for bs in range(2)]
        kTt = [V(TK + 8 + 4 * bs, 4096, BF16) for bs in range(2)]
        KT = [Buf("kT0"), Buf("kT1")]
        NVB = 37
        vt = [V(TK + 16 + 9.5 * bs, NVB * 256, BF16, "p (b n) -> p b n", b=NVB) for bs in range(2)]
        VB = [[Buf(f"v{bs}_{i}") for i in range(NVB)] for bs in range(2)]
        vTt = [V(TK + 45 + 4 * i, 4096, BF16) for i in range(2)]
        VT = [Buf("vT0"), Buf("vT1")]
        sbt = [V(TK + 35 + 2 * i, 2048) for i in range(2)]
        SBT = [Buf("sb0"), Buf("sb1")]
        pTt = [V(TK + 39 + i, 1024, BF16) for i in range(2)]
        PT = [Buf("pT0"), Buf("pT1")]
        rden = [V(TK + 41 + 2 * i, 2048) for i in range(2)]
        RD = [Buf("rd0"), Buf("rd1")]
        stageA_bufs = XT + HB + [SQJ]
        for bs in range(2):
            for hh in range(2):
                S.alias(stageA_bufs, QZ[bs][hh])
            S.alias(stageA_bufs, KT[bs])
            for b_ in VB[bs]:
                S.alias(stageA_bufs, b_)
        for b_ in SBT + PT + RD + VT:
            S.alias(stageA_bufs + [GM], b_)
        if debug == "B01":
            S.op("pool", lambda e: e.memset(mixedT[:, 0, :], 7.0), writes=[MX[0]])
            S.op("pool", lambda e: e.memset(rden[0], 5.0), writes=[RD[0]])
        for bs in range(2):
            for hh in range(2):
                S.op("dve", lambda e, bs=bs, hh=hh: e.memset(qz[bs][hh], 0.0), writes=[QZ[bs][hh]])

        vblocks = []
        v1idx, v4idx, v16idx = {}, {}, {}
        for n in range(7, 16):
            v1idx[n] = len(vblocks)
            vblocks.append((slice(n * 128, (n + 1) * 128), 1 if n == 7 else 0))
        for r4 in range(4):
            for n4 in (1, 2, 3):
                v4idx[(r4, n4)] = len(vblocks)
                vblocks.append((ssl(r4 + 512 * n4, 128, 4), 1 if n4 == 1 else 0))
        for r in range(16):
            v16idx[r] = len(vblocks)
            vblocks.append((ssl(r, 128, 16), 2))
        assert len(vblocks) == NVB
        ALLHT = HT

        def ht_bufs(sl):
            if sl.step is None or sl.step == 1:
                return HT[sl.start // 128:(sl.stop + 127) // 128]
            return ALLHT

        accbank = [0]

        def next_acc():
            k = accbank[0] % 2
            accbank[0] += 1
            return k

        scb = [0]
        evtoggle = [0]

        def evac_copy(dst, src, reads, writes, scale=None):
            evtoggle[0] += 1
            if evtoggle[0] % 2 == 0:
                if scale is None:
                    S.op("act", lambda e: e.activation(out=dst, in_=src, func=AF.Copy), reads=reads, writes=writes)
                else:
                    S.op("act", lambda e: e.activation(out=dst, in_=src, func=AF.Copy, scale=scale), reads=reads, writes=writes)
            else:
                if scale is None:
                    S.op("dve", lambda e: e.tensor_copy(out=dst, in_=src), reads=reads, writes=writes)
                else:
                    S.op("dve", lambda e: e.tensor_scalar(out=dst, in0=src, scalar1=scale, scalar2=None, op0=ALU.mult),
                         reads=reads, writes=writes)

        NUMB = [PB[4], PB[5]]
        DENB = [PB[6], PB[7]]
        pvstep = [0]
        NUMK = [4, 5]
        DENK = [6, 7]

        def inproj_steps(ui, hp):
            ws = ui % 2
            bs = hp % 2
            w = wring[ws]
            for g in range(2):
                k = next_acc()
                for dc in range(NDC):
                    S.op("pe", lambda e, k=k, dc=dc, g=g: e.matmul(banks[k][:], lhsT=w[:, dc, 0:128],
                                                                   rhs=hT[:, dc, OWN + g * 512:OWN + (g + 1) * 512],
                                                                   start=(dc == 0), stop=(dc == NDC - 1)),
                         reads=[WR[ws]] + HT[8 + 4 * g:12 + 4 * g], writes=[PB[k]])
                S.op("act", lambda e, k=k, g=g: e.activation(out=qz[bs][0][0:64, g * 512:(g + 1) * 512], in_=banks[k][0:64, :],
                                                             func=AF.Copy, scale=0.125), reads=[PB[k]], writes=[QZ[bs][0]])
                S.op("act", lambda e, k=k, g=g: e.activation(out=qz[bs][1][64:128, g * 512:(g + 1) * 512], in_=banks[k][64:128, :],
                                                             func=AF.Copy, scale=0.125), reads=[PB[k]], writes=[QZ[bs][1]])
                yield
            for g in range(4):
                k = next_acc()
                for dc in range(NDC):
                    S.op("pe", lambda e, k=k, dc=dc, g=g: e.matmul(banks[k][:], lhsT=w[:, dc, 128:256],
                                                                   rhs=hT[:, dc, g * 512:(g + 1) * 512],
                                                                   start=(dc == 0), stop=(dc == NDC - 1)),
                         reads=[WR[ws]] + HT[4 * g:4 * g + 4], writes=[PB[k]])
                S.op("act", lambda e, k=k, g=g: e.activation(out=kTt[bs][:, g * 512:(g + 1) * 512], in_=banks[k][:], func=AF.Copy),
                     reads=[PB[k]], writes=[KT[bs]])
                yield
            for g in range(4):
                k = next_acc()
                for dc in range(NDC):
                    S.op("pe", lambda e, k=k, dc=dc, g=g: e.matmul(banks[k][:], lhsT=w[:, dc, 256:384],
                                                                   rhs=hT[:, dc, g * 512:(g + 1) * 512],
                                                                   start=(dc == 0), stop=(dc == NDC - 1)),
                         reads=[WR[ws]] + HT[4 * g:4 * g + 4], writes=[PB[k]])
                S.op("act", lambda e, k=k, g=g: e.activation(out=vTt[bs][:, g * 512:(g + 1) * 512], in_=banks[k][:], func=AF.Copy),
                     reads=[PB[k]], writes=[VT[bs]])
                yield
            for b0 in range(0, NVB, 4):
                k = next_acc()
                nb = min(4, NVB - b0)
                pvv = bank_bf(k)[:, 0:512].rearrange("p (j t) -> p j t", j=4)
                for j in range(nb):
                    sl, fk = vblocks[b0 + j]
                    S.op("pe", lambda e, pvv=pvv, j=j, sl=sl: e.transpose(out=pvv[:, j, :], in_=vTt[bs][:, sl], identity=identB),
                         reads=[VT[bs], CB], writes=[PB[k]])
                for j in range(nb):
                    sl, fk = vblocks[b0 + j]
                    src = pvv[:, j, :]
                    dst = vt[bs][:, b0 + j, :]
                    if fk == 0:
                        S.op("act", lambda e, dst=dst, src=src: e.activation(out=dst, in_=src, func=AF.Copy),
                             reads=[PB[k]], writes=[VB[bs][b0 + j]])
                    else:
                        sc = flag if fk == 1 else flag16
                        S.op("act", lambda e, dst=dst, src=src, sc=sc: e.activation(out=dst, in_=src, func=AF.Copy, scale=sc),
                             reads=[PB[k], SM], writes=[VB[bs][b0 + j]])
                yield
            if ui + 2 < len(units):
                issue_unit_dma(ui + 2)

        def attention_steps(ui, hp):
            bs = hp % 2
            kT = kTt[bs]
            flat = []
            for hh in (range(2) if debug not in ("B00", "B01", "B0a") else ((0,) if debug in ("B00", "B0a") else (1,))):
                h = 2 * hp + hh
                slope = 2.0 ** (-8.0 * (h + 1) / 16.0)
                for g in range(2 if debug not in ("B00", "B01") else 1):
                    items = []
                    for half in range(2):
                        mm, pv = [], []
                        for qi in range(2):
                            n = 8 + 4 * g + 2 * half + qi
                            qsl = slice((n - 8) * 128, (n - 7) * 128)
                            ocol = slice((n - 8 - 4 * g) * 128, (n - 7 - 4 * g) * 128)
                            for kb in range(2):
                                nk = n - 1 + kb
                                col = (qi * 2 + kb) * 128
                                mm.append((col, 128, slice(nk * 128, (nk + 1) * 128), qsl))
                                pv.append((col, 128, v1idx[nk], flag64 if nk == 7 else ones64, ocol))
                        items.append((mm, R2, slope * 1.0, pv))
                    for cp in range(2):
                        mm, pv = [], []
                        for ci in range(2):
                            r4 = 2 * cp + ci
                            n4 = 2 + g
                            qsl = ssl(r4 + 512 * g, 128, 4)
                            ocol = ssl(r4, 128, 4)
                            for kb in range(2):
                                nk = n4 - 1 + kb
                                col = (ci * 2 + kb) * 128
                                mm.append((col, 128, ssl(r4 + 512 * nk, 128, 4), qsl))
                                pv.append((col, 128, v4idx[(r4, nk)], flag64 if nk == 1 else ones64, ocol))
                        items.append((mm, R2, slope * 4.0, pv))
                    mm, pv = [], []
                    for r in range(16):
                        qsl = ssl(r + 512 * g, 32, 16)
                        mm.append((r * 32, 32, ssl(r, 128, 16), qsl))
                        pv.append((r * 32, 32, v16idx[r], flag16_64, ssl(r, 32, 16)))
                    items.append((mm, R16[g], slope * 16.0, pv))
                    x = pvstep[0] % 2
                    pvstep[0] += 1
                    for ii, (mm, Rt, scal, pv) in enumerate(items):
                        flat.append(dict(hh=hh, g=g, x=x, mm=mm, Rt=Rt, scal=scal, pv=pv, first=(ii == 0), last=(ii == len(items) - 1)))
            for it in flat:
                it["sk"] = 2 + (scb[0] % 2)
                it["ti"] = scb[0] % 2
                scb[0] += 1

            def emit_scores(it):
                q = qz[bs][it["hh"]]
                sk = it["sk"]
                for col, wdt, ksl, qsl in it["mm"]:
                    S.op("pe", lambda e, sk=sk, col=col, wdt=wdt, ksl=ksl, qsl=qsl, q=q: e.matmul(
                        banks[sk][:, col:col + wdt], lhsT=kT[:, ksl], rhs=q[:, qsl], start=True, stop=True,
                        skip_group_check=True), reads=[KT[bs], QZ[bs][it["hh"]]], writes=[PB[sk]])

            def emit_rest(it):
                sk, ti, x, g = it["sk"], it["ti"], it["x"], it["g"]
                hbp = 64 * it["hh"]
                Rt, scal = it["Rt"], it["scal"]
                S.op("dve", lambda e, sk=sk, ti=ti, Rt=Rt, scal=scal: e.scalar_tensor_tensor(
                    out=sbt[ti], in0=Rt, scalar=float(scal), in1=banks[sk][:], op0=ALU.mult, op1=ALU.add),
                    reads=[PB[sk], CST], writes=[SBT[ti]])
                S.op("act", lambda e, ti=ti: e.activation(out=pTt[ti], in_=sbt[ti], func=AF.Exp),
                     reads=[SBT[ti]], writes=[PT[ti]])
                first = it["first"]
                for col, wdt, vi, denl, ocol in it["pv"]:
                    S.op("pe", lambda e, col=col, wdt=wdt, vi=vi, ocol=ocol, ti=ti, x=x, first=first: e.matmul(
                        banks[NUMK[x]][:, ocol], lhsT=vt[bs][:, vi, :], rhs=pTt[ti][:, col:col + wdt],
                        start=first, stop=True, skip_group_check=True),
                        reads=[VB[bs][vi], PT[ti]], writes=[NUMB[x]])
                    S.op("pe", lambda e, col=col, wdt=wdt, denl=denl, ocol=ocol, ti=ti, x=x, first=first: e.matmul(
                        banks[DENK[x]][:, ocol], lhsT=denl, rhs=pTt[ti][:, col:col + wdt],
                        start=first, stop=True, skip_group_check=True),
                        reads=[CB, PT[ti]], writes=[DENB[x]])
                    first = False
                if it["last"]:
                    ri = x
                    S.op("dve", lambda e, x=x, ri=ri, hbp=hbp: e.reciprocal(out=rden[ri][hbp:hbp + 64, :],
                                                                            in_=banks[DENK[x]][hbp:hbp + 64, :]),
                         reads=[DENB[x]], writes=[RD[ri]])
                    S.op("dve", lambda e, g=g, x=x, ri=ri, hbp=hbp: e.tensor_tensor(
                        out=mixedT[hbp:hbp + 64, hp, g * 512:(g + 1) * 512], in0=banks[NUMK[x]][hbp:hbp + 64, :],
                        in1=rden[ri][hbp:hbp + 64, :], op=ALU.mult), reads=[NUMB[x], RD[ri]], writes=[MX[hp]])

            if flat:
                emit_scores(flat[0])
            for i, it in enumerate(flat):
                if i + 1 < len(flat):
                    emit_scores(flat[i + 1])
                emit_rest(it)
                yield

        def run_all(gen):
            for _ in gen:
                pass

        def interleave(ga, gb, ratio=1):
            da = db = False
            while not (da and db):
                if not da:
                    try:
                        next(ga)
                    except StopIteration:
                        da = True
                for _ in range(ratio):
                    if not db:
                        try:
                            next(gb)
                        except StopIteration:
                            db = True

        n_att = 8 if debug not in ("B0", "B00", "B01", "B0a", "B0x", "B0y") else 1
        issue_unit_dma(1)
        run_all(inproj_steps(0, 0))
        for ui in range(n_att):
            if ui + 1 < n_att:
                interleave(attention_steps(ui, ui), inproj_steps(ui + 1, ui + 1))
            else:
                run_all(attention_steps(ui, ui))

        def dump(ap_list, evs_reads):
            dd = S.dsem()
            evs = []
            for ap, off, n in ap_list:
                evs.append(S.dma("sync", dbg_d[:, off:off + n], ap, dd, reads=evs_reads))
            S.final_wait("sync", evs)
            S.emit()

        if debug in ("B0", "B00", "B01", "B0a", "B0x", "B0y"):
            tmpf = V(0, 32768)
            TMPF = Buf("tmpf")
            S.alias(HT, TMPF)
            S.op("dve", lambda e: e.tensor_copy(out=tmpf[:, 0:1024], in_=qz[0][0]), reads=[QZ[0][0]], writes=[TMPF])
            S.op("dve", lambda e: e.tensor_copy(out=tmpf[:, 1024:2048], in_=qz[0][1]), reads=[QZ[0][1]], writes=[TMPF])
            S.op("dve", lambda e: e.tensor_copy(out=tmpf[:, 2048:4096], in_=kTt[0]), reads=[KT[0]], writes=[TMPF])
            S.op("dve", lambda e: e.tensor_copy(out=tmpf[:, 4096:4096 + 512], in_=vt[0][:, 0:4, :].rearrange("p b n -> p (b n)")), reads=VB[0], writes=[TMPF])
            S.op("dve", lambda e: e.tensor_copy(out=tmpf[:, 4608:5120], in_=banks[4][:]), reads=[NUMB[0]], writes=[TMPF])
            S.op("dve", lambda e: e.tensor_copy(out=tmpf[:, 5120:5632], in_=banks[6][:]), reads=[DENB[0]], writes=[TMPF])
            S.op("dve", lambda e: e.tensor_copy(out=tmpf[:, 5632:6144], in_=pTt[0]), reads=[PT[0]], writes=[TMPF])
            S.op("dve", lambda e: e.tensor_copy(out=tmpf[:, 6144:6656], in_=rden[0]), reads=[RD[0]], writes=[TMPF])
            S.op("dve", lambda e: e.tensor_copy(out=tmpf[:, 6656:7680], in_=mixedT[:, 0, :]), reads=[MX[0]], writes=[TMPF])
            S.op("dve", lambda e: e.tensor_copy(out=tmpf[:, 7680:8192], in_=sbt[0]), reads=[SBT[0]], writes=[TMPF])
            dump([(tmpf, 0, 8192)], [TMPF])
            return nc
        if debug == "attn":
            tmpf = V(TK, 32768)
            TMPF = Buf("tmpf")
            S.alias(stageA_bufs + [b for bs in range(2) for b in VB[bs]] + KT + SBT + PT + RD + [QZ[a][b] for a in range(2) for b in range(2)], TMPF)
            S.op("dve", lambda e: e.tensor_copy(out=tmpf, in_=mixedT[:, 0:8, :].rearrange("p c t -> p (c t)")), reads=MX[0:8], writes=[TMPF])
            dump([(tmpf, 0, 8192)], [TMPF])
            return nc

        stageB_bufs = [b for bs in range(2) for b in VB[bs]] + KT + SBT + PT + RD + VT + [QZ[a][b] for a in range(2) for b in range(2)]
        xraw2 = [V(TK + 8.25 * i, 8448) for i in range(2)]
        gg2 = [V(TK + 16.5 + 4 * i, 4096) for i in range(2)]
        xc = V(TK + 24.5, 8192)
        ra = V(TK + 32.5, 4096)
        ix = V(TK + 36.5, 4096)
        at = V(TK + 40.5, 4096)
        tt = V(TK + 44.5, 4096)
        hh_ = V(TK + 48.5, 4096)
        xs = V(TK + 52.5, 2048)
        uu = V(TK + 54.5, 2048)
        wabd = [V(TK + 56.5 + i, 1024) for i in range(2)]
        XC, RA, IX, AT, TT, HH, XS, UU = (Buf(n) for n in ("xc", "ra", "ix", "at", "tt", "hh", "xs", "uu"))
        XRAW2 = [Buf("xraw0"), Buf("xraw1")]
        GG2 = [Buf("gg0"), Buf("gg1")]
        WAB = [Buf("wab0"), Buf("wab1")]
        dwab = [S.dsem(), S.dsem()]
        stageC_list = [XC, RA, IX, AT, TT, HH, XS, UU] + XRAW2 + GG2 + WAB
        for b_ in stageC_list:
            S.alias(stageA_bufs + stageB_bufs + [GM], b_)
        for i in range(2):
            S.op("dve", lambda e, i=i: e.memset(xraw2[i][:, 0:3], 0.0), writes=[XRAW2[i]])

        def lru_inproj_steps(ui, cc):
            ws = ui % 2
            w = wring[ws]
            xraw = xraw2[cc % 2]
            XRAW = XRAW2[cc % 2]
            gg = gg2[cc % 2]
            GG = GG2[cc % 2]
            S.dma("sync", wabd[cc % 2], wabd_d[cc], dwab[cc % 2], writes=[WAB[cc % 2]])
            for g in range(4):
                k = next_acc()
                for dc in range(NDC):
                    S.op("pe", lambda e, k=k, dc=dc, g=g: e.matmul(banks[k][:], lhsT=w[:, dc, 0:128], rhs=hT[:, dc, g * 512:(g + 1) * 512],
                                                                   start=(dc == 0), stop=(dc == NDC - 1)),
                         reads=[WR[ws]] + HT[4 * g:4 * g + 4], writes=[PB[k]])
                S.op("act", lambda e, k=k, g=g: e.activation(out=xraw[:, 3 + g * 512:3 + (g + 1) * 512], in_=banks[k][:], func=AF.Copy),
                     reads=[PB[k]], writes=[XRAW])
                yield
            for g in range(2):
                k = next_acc()
                for dc in range(NDC):
                    S.op("pe", lambda e, k=k, dc=dc, g=g: e.matmul(banks[k][:], lhsT=w[:, dc, 128:256],
                                                                   rhs=hT[:, dc, OWN + g * 512:OWN + (g + 1) * 512],
                                                                   start=(dc == 0), stop=(dc == NDC - 1)),
                         reads=[WR[ws]] + HT[8 + 4 * g:12 + 4 * g], writes=[PB[k]])
                S.op("act", lambda e, k=k: e.activation(out=xs, in_=banks[k][:], func=AF.Copy), reads=[PB[k]], writes=[XS])
                S.op("act", lambda e, k=k: e.activation(out=uu, in_=banks[k][:], func=AF.Square), reads=[PB[k]], writes=[UU])
                S.op("dve", lambda e: e.tensor_scalar(out=uu, in0=uu, scalar1=0.044715, scalar2=1.0, op0=ALU.mult, op1=ALU.add),
                     reads=[UU], writes=[UU])
                S.op("dve", lambda e: e.tensor_tensor(out=uu, in0=uu, in1=xs, op=ALU.mult), reads=[UU, XS], writes=[UU])
                S.op("act", lambda e: e.activation(out=uu, in_=uu, func=AF.Sigmoid, scale=1.5957691216057308), reads=[UU], writes=[UU])
                S.op("dve", lambda e, g=g: e.tensor_tensor(out=gg[:, g * 512:(g + 1) * 512], in0=uu, in1=xs, op=ALU.mult),
                     reads=[UU, XS], writes=[GG])
                yield
            if ui + 2 < len(units):
                issue_unit_dma(ui + 2)

        def lru_chain_steps(ui, cc):
            xraw = xraw2[cc % 2]
            XRAW = XRAW2[cc % 2]
            gg = gg2[cc % 2]
            GG = GG2[cc % 2]
            wa = wabd[cc % 2]
            cw = lambda i: sm[:, SM_CONVW + cc * 4 + i:SM_CONVW + cc * 4 + i + 1]
            S.op("dve", lambda e: e.tensor_scalar(out=xc, in0=xraw[:, 0:WIN], scalar1=cw(0), scalar2=sm[:, SM_CONVB + cc:SM_CONVB + cc + 1],
                                                  op0=ALU.mult, op1=ALU.add), reads=[XRAW, SM], writes=[XC])
            for i in range(1, 4):
                S.op("dve", lambda e, i=i: e.scalar_tensor_tensor(out=xc, in0=xraw[:, i:i + WIN], scalar=cw(i), in1=xc,
                                                                  op0=ALU.mult, op1=ALU.add), reads=[XRAW, SM, XC], writes=[XC])
            yield
            for hf in range(2):
                for g2 in range(2):
                    t0 = hf * 1024 + g2 * 512
                    for which, dstb, DB, bcol in ((0, ra, RA, SM_BA), (1, ix, IX, SM_BX)):
                        k = next_acc()
                        S.op("pe", lambda e, k=k, which=which, t0=t0: e.matmul(banks[k][:], lhsT=wa[:, which * 128:(which + 1) * 128],
                                                                               rhs=xc[:, t0:t0 + 512], start=True, stop=True),
                             reads=[WAB[cc % 2], XC], writes=[PB[k]])
                        S.op("act", lambda e, k=k, dstb=dstb, g2=g2, bcol=bcol: e.activation(
                            out=dstb[:, g2 * 512:(g2 + 1) * 512], in_=banks[k][:], func=AF.Sigmoid,
                            bias=sm[:, bcol + cc:bcol + cc + 1]), reads=[PB[k], SM], writes=[DB])
                    yield
                S.op("act", lambda e: e.activation(out=at, in_=ra, func=AF.Exp, scale=c_lru[:, cc:cc + 1]), reads=[RA, SM], writes=[AT])
                S.op("act", lambda e: e.activation(out=tt, in_=ra, func=AF.Exp, scale=c2_lru[:, cc:cc + 1]), reads=[RA, SM], writes=[TT])
                S.op("act", lambda e: e.activation(out=tt, in_=tt, func=AF.Sqrt, scale=-1.0, bias=1.0), reads=[TT], writes=[TT])
                S.op("dve", lambda e: e.tensor_tensor(out=tt, in0=tt, in1=ix, op=ALU.mult), reads=[TT, IX], writes=[TT])
                S.op("dve", lambda e, hf=hf: e.tensor_tensor(out=tt, in0=tt, in1=xc[:, hf * 1024:(hf + 1) * 1024], op=ALU.mult),
                     reads=[TT, XC], writes=[TT])
                if hf == 0:
                    S.op("dve", lambda e: e.tensor_tensor_scan(out=hh_, data0=at, data1=tt, initial=0.0, op0=ALU.mult, op1=ALU.add),
                         reads=[AT, TT], writes=[HH])
                    S.op("dve", lambda e: e.tensor_tensor(out=s0t, in0=hh_[:, 1023:1024], in1=flag, op=ALU.mult),
                         reads=[HH, SM], writes=[SM])
                else:
                    S.op("dve", lambda e: e.tensor_tensor_scan(out=hh_, data0=at, data1=tt, initial=s0t, op0=ALU.mult, op1=ALU.add),
                         reads=[AT, TT, SM], writes=[HH])
                    S.op("dve", lambda e: e.tensor_tensor(out=mixedT[:, 8 + cc, :], in0=hh_, in1=gg, op=ALU.mult),
                         reads=[HH, GG], writes=[MX[8 + cc]])
                yield

        run_all(lru_inproj_steps(8, 0))
        for cc in range(8):
            if cc + 1 < 8:
                interleave(lru_chain_steps(8 + cc, cc), lru_inproj_steps(9 + cc, cc + 1))
            else:
                run_all(lru_chain_steps(8 + cc, cc))

        if debug == "mixed":
            tmpf = V(0, 65536)
            TMPF = Buf("tmpf")
            S.alias(HT + WR, TMPF)
            S.op("dve", lambda e: e.tensor_copy(out=tmpf, in_=mixedT.rearrange("p c t -> p (c t)")), reads=MX, writes=[TMPF])
            dump([(tmpf, 0, 16384)], [TMPF])
            return nc

        stageC_bufs = stageC_list
        p1_T = stageA_bufs + stageB_bufs + stageC_bufs
        x2 = V(0, 65536, F32, "p (c d) -> p c d", c=8)
        X2 = [Buf(f"x2_{t}") for t in range(8)]
        for b_ in X2:
            S.alias(HT, b_)
        dx2 = S.dsem()
        for t in range(8):
            S.dma("sync", x2[:, t, :], xw[OWN + t * 128:OWN + (t + 1) * 128, :], dx2, writes=[X2[t]])
        for b_ in X2:
            b_.lw = (dx2.key, dx2.count)
        wo = [V(96 + 16 * i, 16384, BF16, "p (c n) -> p c n", c=NDC) for i in range(2)]
        WO = [Buf("wo0"), Buf("wo1")]
        dwo = [S.dsem(), S.dsem()]
        for b_ in WO:
            S.alias(WR + p1_T, b_)
        woutv = wout_d.rearrange("(c p) n -> p c n", p=128)

        def issue_wo(n4):
            S.dma("sync", wo[n4 % 2].rearrange("p c n -> p (c n)"), wo_bf[n4], dwo[n4 % 2], reads=[PCB[("wo", 0, n4)]],
                  writes=[WO[n4 % 2]])
        issue_wo(0)
        sqt = [V(128 + 2 * i, 2048) for i in range(2)]
        SQT = [Buf("sqt0"), Buf("sqt1")]
        rstd_bc = V(132, 8192, F32, "p (g t) -> p g t", g=2)
        RSTD = [Buf("rstd0"), Buf("rstd1")]
        for b_ in SQT + RSTD:
            S.alias(p1_T, b_)
        for grp in range(2):
            for g in range(2):
                k = grp * 2 + g
                for ci in range(8):
                    c = grp * 8 + ci
                    ti = ci % 2
                    S.op("act", lambda e, c=c, g=g, ti=ti: e.activation(out=sqt[ti], in_=mixedT[:, c, g * 512:(g + 1) * 512], func=AF.Square),
                         reads=[MX[c]], writes=[SQT[ti]])
                    S.op("pe", lambda e, k=k, ti=ti, ci=ci: e.matmul(banks[k][:], lhsT=onesF, rhs=sqt[ti], start=(ci == 0), stop=(ci == 7)),
                         reads=[SQT[ti], CB], writes=[PB[k]])
                dst = rstd_bc[:, grp, g * 512:(g + 1) * 512]
                S.op("dve", lambda e, k=k, dst=dst: e.tensor_scalar(out=dst, in0=banks[k][:], scalar1=1.0 / 1024, scalar2=EPS,
                                                                    op0=ALU.mult, op1=ALU.add), reads=[PB[k]], writes=[RSTD[grp]])
                S.op("act", lambda e, dst=dst: e.activation(out=dst, in_=dst, func=AF.Sqrt), reads=[RSTD[grp]], writes=[RSTD[grp]])
                S.op("dve", lambda e, dst=dst: e.reciprocal(out=dst, in_=dst), reads=[RSTD[grp]], writes=[RSTD[grp]])
        for c in range(16):
            grp = c // 8
            S.op("dve", lambda e, c=c, grp=grp: e.scalar_tensor_tensor(out=mixedT[:, c, :], in0=mixedT[:, c, :],
                                                                       scalar=sm[:, SM_GMIXT + c:SM_GMIXT + c + 1],
                                                                       in1=rstd_bc[:, grp, :], op0=ALU.mult, op1=ALU.mult),
                 reads=[MX[c], SM, RSTD[grp]], writes=[MX[c]])
        opb = [0]
        for n4 in range(4):
            if n4 + 1 < 4:
                issue_wo(n4 + 1)
            s = n4 % 2
            for t in range(8):
                k = 4 + (opb[0] % 4)
                opb[0] += 1
                for kc in range(16):
                    S.op("pe", lambda e, k=k, kc=kc, t=t, s=s: e.matmul(banks[k][:], lhsT=mixedT[:, kc, t * 128:(t + 1) * 128],
                                                                        rhs=wo[s][:, kc, :], start=(kc == 0), stop=(kc == 15)),
                         reads=[MX[kc], WO[s]], writes=[PB[k]])
                S.op("dve", lambda e, k=k, t=t, n4=n4: e.tensor_tensor(out=x2[:, t, n4 * 512:(n4 + 1) * 512], in0=banks[k][:],
                                                                       in1=x2[:, t, n4 * 512:(n4 + 1) * 512], op=ALU.add),
                     reads=[PB[k], X2[t]], writes=[X2[t]])

        if debug == "x2":
            dump([(x2.rearrange("p c d -> p (c d)"), 0, 16384)], X2)
            return nc

        x2nb = V(64, 32768, BF16, "p (c d) -> p c d", c=8)
        X2N = [Buf(f"x2n{t}") for t in range(8)]
        for b_ in X2N:
            S.alias(MX, b_)
        gbc = V(175, 8192)
        GBC = Buf("gbc")
        S.alias([GM] + p1_T, GBC)
        dgb = S.dsem()
        S.dma("sync", gbc, gffn_d, dgb, writes=[GBC])
        x2nf = V(128, 8192)
        X2NF = Buf("x2nf")
        xTc = V(136, 8192, F32, "p (c t) -> p c t", c=NDC)
        XTC = Buf("xTc")
        wrt = V(144, NDC * NE * 4, F32, "p (c n) -> p c n", c=NDC)
        WRT = Buf("wrt")
        S.alias(SQT + RSTD + p1_T, X2NF)
        S.alias(SQT + RSTD + p1_T, XTC)
        S.alias(SQT + RSTD + p1_T + WO, WRT)
        dwrt = S.dsem()
        S.dma("sync", wrt, wrt_d, dwrt, writes=[WRT])
        posf = V(146, 1024, F32, "p (t e) -> p t e", t=8)
        maskf = V(147, 1024, F32, "p (t e) -> p t e", t=8)
        gatef = V(148, 1024, F32, "p (t e) -> p t e", t=8)
        gHL = V(149, 1024, BF16, "p (t e two) -> p t e two", t=8, two=2)
        maskb = V(150, 512, BF16, "p (t e) -> p t e", t=8)
        logit = V(150.5, 128)
        mx8 = V(150.625, 32)
        exq = V(150.75, 128)
        misc = V(150.875, 64)
        GTp = V(151, 4096)
        bdp = V(159, 8192)
        ROUT = [Buf(f"rout{t}") for t in range(8)]
        RTMP = Buf("rtmp")
        GTB, BDP = Buf("gtp"), Buf("bdp")
        for b_ in ROUT + [RTMP, GTB, BDP]:
            S.alias(SQT + RSTD + p1_T + WO, b_)
        S.op("dve", lambda e: e.memset(GTp, 0.0), writes=[GTB])
        S.op("dve", lambda e: e.memset(bdp, 0.0), writes=[BDP])
        dbd = S.dsem()
        S.dma("sync", bdp[0:NE, :], bdn_d, dbd, reads=[], writes=[BDP])
        ssq2 = small_t[:, 32:40]
        rs2 = small_t[:, 40:48]
        tpb = [0]
        for t in range(8):
            S.op("act", lambda e, t=t: e.activation(out=x2nf, in_=x2[:, t, :], func=AF.Square, accum_out=ssq2[:, t:t + 1]),
                 reads=[X2[t]], writes=[X2NF, STAT])
            S.op("dve", lambda e, t=t: e.tensor_scalar(out=rs2[:, t:t + 1], in0=ssq2[:, t:t + 1], scalar1=1.0 / D, scalar2=EPS,
                                                       op0=ALU.mult, op1=ALU.add), reads=[STAT], writes=[STAT])
            S.op("act", lambda e, t=t: e.activation(out=rs2[:, t:t + 1], in_=rs2[:, t:t + 1], func=AF.Sqrt), reads=[STAT], writes=[STAT])
            S.op("dve", lambda e, t=t: e.reciprocal(out=rs2[:, t:t + 1], in_=rs2[:, t:t + 1]), reads=[STAT], writes=[STAT])
            S.op("dve", lambda e, t=t: e.scalar_tensor_tensor(out=x2nf, in0=x2[:, t, :], scalar=rs2[:, t:t + 1], in1=gbc,
                                                              op0=ALU.mult, op1=ALU.mult), reads=[X2[t], STAT, GBC, X2NF], writes=[X2NF])
            S.op("act", lambda e, t=t: e.activation(out=x2nb[:, t, :], in_=x2nf, func=AF.Copy), reads=[X2NF], writes=[X2N[t]])
            for q4 in range(4):
                k = tpb[0] % 4
                tpb[0] += 1
                for j in range(4):
                    dc = q4 * 4 + j
                    S.op("pe", lambda e, k=k, j=j, dc=dc: e.transpose(out=banks[k][:, j * 128:(j + 1) * 128],
                                                                      in_=x2nf[:, dc * 128:(dc + 1) * 128], identity=identF),
                         reads=[X2NF, CST], writes=[PB[k]])
                evac_copy(xTc[:, q4 * 4:q4 * 4 + 4, :], banks[k][:].rearrange("p (j t) -> p j t", j=4), [PB[k]], [XTC])
            for dc in range(NDC):
                S.op("pe", lambda e, dc=dc: e.matmul(banks[4][:, 0:NE], lhsT=xTc[:, dc, :], rhs=wrt[:, dc, :],
                                                     start=(dc == 0), stop=(dc == NDC - 1)), reads=[XTC, WRT], writes=[PB[4]])
            S.op("dve", lambda e: e.tensor_tensor(out=logit, in0=banks[4][:, 0:NE], in1=sm[:, SM_BROUT:SM_BROUT + NE], op=ALU.add),
                 reads=[PB[4], SM], writes=[RTMP])
            S.op("dve", lambda e: e.max(out=mx8, in_=logit), reads=[RTMP], writes=[RTMP])
            S.op("dve", lambda e, t=t: e.tensor_scalar(out=maskf[:, t, :], in0=logit, scalar1=mx8[:, 3:4], scalar2=None, op0=ALU.is_ge),
                 reads=[RTMP], writes=[ROUT[t]])
            S.op("dve", lambda e: e.tensor_scalar(out=misc[:, 0:1], in0=mx8[:, 0:1], scalar1=-1.0, scalar2=None, op0=ALU.mult),
                 reads=[RTMP], writes=[RTMP])
            S.op("act", lambda e: e.activation(out=exq, in_=logit, func=AF.Exp, bias=misc[:, 0:1]), reads=[RTMP], writes=[RTMP])
            S.op("dve", lambda e, t=t: e.tensor_tensor(out=exq, in0=exq, in1=maskf[:, t, :], op=ALU.mult), reads=[RTMP, ROUT[t]], writes=[RTMP])
            S.op("dve", lambda e: e.reduce_sum(out=misc[:, 1:2], in_=exq, axis=mybir.AxisListType.X), reads=[RTMP], writes=[RTMP])
            S.op("dve", lambda e: e.reciprocal(out=misc[:, 1:2], in_=misc[:, 1:2]), reads=[RTMP], writes=[RTMP])
            S.op("dve", lambda e, t=t: e.tensor_scalar(out=gatef[:, t, :], in0=exq, scalar1=misc[:, 1:2], scalar2=None, op0=ALU.mult),
                 reads=[RTMP, ROUT[t]], writes=[ROUT[t]])
            S.op("dve", lambda e, t=t: e.tensor_copy(out=gHL[:, t, :, 0], in_=gatef[:, t, :]), reads=[ROUT[t]], writes=[ROUT[t]])
            S.op("dve", lambda e, t=t: e.tensor_tensor(out=gHL[:, t, :, 1], in0=gatef[:, t, :], in1=gHL[:, t, :, 0], op=ALU.subtract),
                 reads=[ROUT[t]], writes=[ROUT[t]])
            S.op("dve", lambda e, t=t: e.tensor_copy(out=maskb[:, t, :], in_=maskf[:, t, :]), reads=[ROUT[t]], writes=[ROUT[t]])
            for tp in range(t + 1):
                S.op("pe", lambda e, tp=tp, t=t: e.matmul(banks[5][:, 0:NE], lhsT=(onesB if tp < t else ltriB), rhs=maskb[:, tp, :],
                                                          start=(tp == 0), stop=(tp == t)),
                     reads=[ROUT[tp], CB], writes=[PB[5]])
            S.op("act", lambda e, t=t: e.activation(out=posf[:, t, :], in_=banks[5][:, 0:NE], func=AF.Copy), reads=[PB[5]], writes=[ROUT[t]])
            S.op("pe", lambda e, t=t: e.transpose(out=banks[6][0:NE, 0:128], in_=gatef[:, t, :], identity=identF),
                 reads=[ROUT[t], CST], writes=[PB[6]])
            S.op("act", lambda e, t=t: e.activation(out=GTp[0:NE, t * 128:(t + 1) * 128], in_=banks[6][0:NE, 0:128], func=AF.Copy),
                 reads=[PB[6]], writes=[GTB])
        for t in range(8):
            for dgp in range(4):
                k = 4 + (opb[0] % 4)
                opb[0] += 1
                S.op("pe", lambda e, k=k, t=t, dgp=dgp: e.matmul(banks[k][:], lhsT=GTp[:, t * 128:(t + 1) * 128],
                                                                 rhs=bdp[:, dgp * 512:(dgp + 1) * 512], start=True, stop=True),
                     reads=[GTB, BDP], writes=[PB[k]])
                S.op("dve", lambda e, k=k, t=t, dgp=dgp: e.tensor_tensor(out=x2[:, t, dgp * 512:(dgp + 1) * 512], in0=banks[k][:],
                                                                         in1=x2[:, t, dgp * 512:(dgp + 1) * 512], op=ALU.add),
                     reads=[PB[k], X2[t]], writes=[X2[t]])

        if debug == "route":
            tmpf = V(159, 16384)
            TMPF = Buf("tmpf")
            S.alias(p1_T + WO + SQT + RSTD, TMPF)
            S.op("dve", lambda e: e.tensor_copy(out=tmpf[:, 0:256], in_=gatef.rearrange("p t e -> p (t e)")), reads=ROUT, writes=[TMPF])
            S.op("dve", lambda e: e.tensor_copy(out=tmpf[:, 256:512], in_=posf.rearrange("p t e -> p (t e)")), reads=ROUT, writes=[TMPF])
            S.op("dve", lambda e: e.tensor_copy(out=tmpf[:, 512:768], in_=maskf.rearrange("p t e -> p (t e)")), reads=ROUT, writes=[TMPF])
            dump([(tmpf[:, 0:768], 0, 768)], [TMPF])
            return nc

        ring = [V(96 + 16 * i, 16384, BF16) for i in range(NRING)]
        RING = [Buf(f"ring{i}") for i in range(NRING)]
        dring = [S.dsem() for _ in range(NRING)]
        for b_ in RING:
            S.alias(WO + WR + p1_T + SQT + RSTD + [X2NF, XTC, WRT], b_)
        xg = V(151, 8192, BF16, "p (c s) -> p c s", c=NDC)
        actT = V(159, 8192, BF16, "p (c s) -> p c s", c=16)
        Sel = V(167, 4096, BF16, "p (t s) -> p t s", t=8)
        SelT = V(171, 4096, BF16, "p (b t) -> p b t", b=2)
        ysl = V(175, 8192, BF16, "p (b d) -> p b d", b=2)
        gc_t = [V(183 + i, 1024) for i in range(2)]
        sg_t = [V(185 + i, 1024) for i in range(2)]
        u1_t = [V(187 + i, 1024) for i in range(2)]
        gsl = small_t[:, 64:66]
        XG, ACTT, SEL, SELT = Buf("xg"), Buf("actT"), Buf("Sel"), Buf("SelT")
        YSL = [Buf("ysl0"), Buf("ysl1"), Buf("ysl2"), Buf("ysl3")]
        EW = [Buf("ew0"), Buf("ew1")]
        for b_ in [XG, ACTT, SEL, SELT] + YSL + EW:
            S.alias(p1_T + SQT + RSTD + [X2NF, XTC, WRT, GBC, BDP, GTB], b_)
        issue_precast(1000)
        NEXP = NE if debug != "moe1" else 1
        ex_order = list(range(NE)) if NEXP == NE else [0]
        pieces = []
        for ex in ex_order:
            for pc in range(8):
                pieces.append(("gu", ex, pc))
            for dgp in range(4):
                pieces.append(("dn", ex, dgp))

        def issue_piece(pi):
            kind, ex, j = pieces[pi]
            s = pi % NRING
            if (kind, ex, j) in PCB:
                S.dma("sync", ring[s], wgu_bf[ex, j // 3], dring[s], reads=[PCB[(kind, ex, j)]], writes=[RING[s]])
            else:
                src = wgu_d[ex, j] if kind == "gu" else wdn_d[ex, j]
                S.dma("pool", ring[s].rearrange("p (a b) -> p a b", b=2048), src.rearrange("p (a b) -> p a b", b=2048),
                      dring[s], writes=[RING[s]])
        for pi in range(NRING):
            issue_piece(pi)
        ga = [0]
        gu = [0]
        dnb = [0]
        scb2 = [0]
        ewi = [0]
        pidx = [0]
        for ex in ex_order:
            for t in range(8):
                S.op("dve", lambda e, t=t, ex=ex: e.tensor_scalar(out=Sel[:, t, :], in0=iota, scalar1=posf[:, t, ex:ex + 1],
                                                                  scalar2=maskf[:, t, ex:ex + 1], op0=ALU.is_equal, op1=ALU.mult),
                     reads=[CST, ROUT[t]], writes=[SEL])
            for dcp in range(8):
                k = ga[0] % 2
                ga[0] += 1
                for j in range(2):
                    dc = 2 * dcp + j
                    for t in range(8):
                        S.op("pe", lambda e, k=k, j=j, dc=dc, t=t: e.matmul(banks[k][:, j * 256:(j + 1) * 256],
                                                                            lhsT=x2nb[:, t, dc * 128:(dc + 1) * 128], rhs=Sel[:, t, :],
                                                                            start=(t == 0), stop=(t == 7), skip_group_check=True),
                             reads=[X2N[t], SEL], writes=[PB[k]])
                evac_copy(xg[:, 2 * dcp:2 * dcp + 2, :], banks[k][:].rearrange("p (j s) -> p j s", j=2), [PB[k]], [XG])
            for sb in range(2):
                for tq in range(2):
                    k = ga[0] % 2
                    ga[0] += 1
                    pv = bank_bf(k)[:, 0:512].rearrange("p (j t) -> p j t", j=4)
                    for j in range(4):
                        t = 4 * tq + j
                        S.op("pe", lambda e, pv=pv, j=j, t=t, sb=sb: e.transpose(out=pv[:, j, :], in_=Sel[:, t, sb * 128:(sb + 1) * 128],
                                                                                identity=identB),
                             reads=[SEL, CB], writes=[PB[k]])
                    evac_copy(SelT[:, sb, tq * 512:(tq + 1) * 512], bank_bf(k)[:, 0:512], [PB[k]], [SELT])
            k = ga[0] % 2
            ga[0] += 1
            for sb in range(2):
                for t in range(8):
                    S.op("pe", lambda e, k=k, sb=sb, t=t, ex=ex: e.matmul(banks[k][:, sb * 2:sb * 2 + 2], lhsT=Sel[:, t, sb * 128:(sb + 1) * 128],
                                                                          rhs=gHL[:, t, ex, :], start=(t == 0), stop=(t == 7),
                                                                          skip_group_check=True),
                         reads=[SEL, ROUT[t]], writes=[PB[k]])
            S.op("dve", lambda e, k=k: e.tensor_reduce(out=gsl, in_=banks[k][:, 0:4].rearrange("p (b two) -> p b two", two=2),
                                                       axis=mybir.AxisListType.X, op=ALU.add), reads=[PB[k]], writes=[STAT])
            for pc in range(8):
                pi = pidx[0]
                pidx[0] += 1
                s = pi % NRING
                wv = ring[s].rearrange("p (c n) -> p c n", c=NDC)
                for fcj in range(2):
                    fc = 2 * pc + fcj
                    k = 2 + (gu[0] % 2)
                    gu[0] += 1
                    for half in range(2):
                        for dc in range(NDC):
                            S.op("pe", lambda e, k=k, half=half, dc=dc, fcj=fcj, wv=wv: e.matmul(
                                banks[k][:, half * 256:(half + 1) * 256],
                                lhsT=wv[:, dc, half * 256 + fcj * 128:half * 256 + (fcj + 1) * 128], rhs=xg[:, dc, :],
                                start=(dc == 0), stop=(dc == NDC - 1), skip_group_check=True),
                                reads=[RING[s], XG], writes=[PB[k]])
                    i = ewi[0] % 2
                    ewi[0] += 1
                    bg = bgu[:, ex * 32 + fc:ex * 32 + fc + 1]
                    bu = bgu[:, ex * 32 + 16 + fc:ex * 32 + 16 + fc + 1]
                    S.op("dve", lambda e, k=k, i=i, bg=bg: e.tensor_scalar(out=gc_t[i], in0=banks[k][:, 0:256], scalar1=bg, scalar2=7.0,
                                                                           op0=ALU.add, op1=ALU.min), reads=[PB[k], BGU], writes=[EW[i]])
                    S.op("act", lambda e, i=i: e.activation(out=sg_t[i], in_=gc_t[i], func=AF.Sigmoid, scale=1.702),
                         reads=[EW[i]], writes=[EW[i]])
                    S.op("dve", lambda e, k=k, i=i, bu=bu: e.tensor_scalar(out=u1_t[i], in0=banks[k][:, 256:512], scalar1=bu, scalar2=8.0,
                                                                           op0=ALU.add, op1=ALU.min), reads=[PB[k], BGU], writes=[EW[i]])
                    S.op("dve", lambda e, i=i: e.tensor_tensor(out=sg_t[i], in0=sg_t[i], in1=gc_t[i], op=ALU.mult),
                         reads=[EW[i]], writes=[EW[i]])
                    S.op("dve", lambda e, i=i, fc=fc: e.scalar_tensor_tensor(out=actT[:, fc, :], in0=u1_t[i], scalar=-6.0, in1=sg_t[i],
                                                                             op0=ALU.max, op1=ALU.mult), reads=[EW[i]], writes=[ACTT])
                if pi + NRING < len(pieces):
                    issue_piece(pi + NRING)
            for dgp in range(4):
                pi = pidx[0]
                pidx[0] += 1
                s = pi % NRING
                wv = ring[s].rearrange("p (c n) -> p c n", c=16)
                for sb in range(2):
                    k = 4 + (dnb[0] % 2)
                    dnb[0] += 1
                    for fc in range(16):
                        S.op("pe", lambda e, k=k, fc=fc, sb=sb, wv=wv: e.matmul(banks[k][:], lhsT=actT[:, fc, sb * 128:(sb + 1) * 128],
                                                                                rhs=wv[:, fc, :], start=(fc == 0), stop=(fc == 15)),
                             reads=[ACTT, RING[s]], writes=[PB[k]])
                    S.op("act", lambda e, k=k, sb=sb, dgp=dgp: e.activation(out=ysl[:, sb, dgp * 512:(dgp + 1) * 512], in_=banks[k][:],
                                                                            func=AF.Copy, scale=gsl[:, sb:sb + 1]),
                         reads=[PB[k], STAT], writes=[YSL[dgp]])
                if pi + NRING < len(pieces):
                    issue_piece(pi + NRING)
                for t in range(8):
                    k = 6 + (scb2[0] % 2)
                    scb2[0] += 1
                    for sb in range(2):
                        S.op("pe", lambda e, k=k, sb=sb, t=t, dgp=dgp: e.matmul(banks[k][:], lhsT=SelT[:, sb, t * 128:(t + 1) * 128],
                                                                                rhs=ysl[:, sb, dgp * 512:(dgp + 1) * 512],
                                                                                start=(sb == 0), stop=(sb == 1)),
                             reads=[SELT, YSL[dgp]], writes=[PB[k]])
                    S.op("dve", lambda e, k=k, t=t, dgp=dgp: e.tensor_tensor(out=x2[:, t, dgp * 512:(dgp + 1) * 512], in0=banks[k][:],
                                                                             in1=x2[:, t, dgp * 512:(dgp + 1) * 512], op=ALU.add),
                         reads=[PB[k], X2[t]], writes=[X2[t]])

        if debug in ("moe1", "x3"):
            dump([(x2.rearrange("p c d -> p (c d)"), 0, 16384)], X2)
            return nc

        dgf = S.dsem()
        S.alias(EW + YSL + [XG, ACTT, SEL, SELT], GBC)
        S.dma("sync", gbc, gfin_d, dgf, writes=[GBC])
        ot = [V(128 + 8 * i, 8192) for i in range(2)]
        OT = [Buf("ot0"), Buf("ot1")]
        for b_ in OT:
            S.alias(RING + [X2NF, XTC, WRT, GTB, BDP] + ROUT + [RTMP], b_)
        dout = [S.dsem(), S.dsem()]
        ssq3 = small_t[:, 48:56]
        rs3 = small_t[:, 56:64]
        evs = []
        for t in range(8):
            i = t % 2
            S.op("act", lambda e, t=t, i=i: e.activation(out=ot[i], in_=x2[:, t, :], func=AF.Square, accum_out=ssq3[:, t:t + 1]),
                 reads=[X2[t]], writes=[OT[i], STAT])
            S.op("dve", lambda e, t=t: e.tensor_scalar(out=rs3[:, t:t + 1], in0=ssq3[:, t:t + 1], scalar1=1.0 / D, scalar2=EPS,
                                                       op0=ALU.mult, op1=ALU.add), reads=[STAT], writes=[STAT])
            S.op("act", lambda e, t=t: e.activation(out=rs3[:, t:t + 1], in_=rs3[:, t:t + 1], func=AF.Sqrt), reads=[STAT], writes=[STAT])
            S.op("dve", lambda e, t=t: e.reciprocal(out=rs3[:, t:t + 1], in_=rs3[:, t:t + 1]), reads=[STAT], writes=[STAT])
            S.op("dve", lambda e, t=t, i=i: e.scalar_tensor_tensor(out=ot[i], in0=x2[:, t, :], scalar=rs3[:, t:t + 1], in1=gbc,
                                                                   op0=ALU.mult, op1=ALU.mult), reads=[X2[t], STAT, GBC, OT[i]], writes=[OT[i]])
            evs.append(S.dma("sync", out_d[t * 128:(t + 1) * 128, :], ot[i], dout[i], reads=[OT[i]]))
        S.final_wait("sync", evs)
        S.emit()
    return nc


def prep_shared(inp):
    f = lambda a: np.ascontiguousarray(np.asarray(a, dtype=np.float32))
    w_in = f(inp["w_in"])
    sh = {}
    sh["cst"] = _make_cst()
    sh["gmix_bc"] = f(np.broadcast_to(inp["norm_mix"][None, :], (128, D)))
    sh["gffn_bc"] = f(np.broadcast_to(inp["norm_ffn"][None, :], (128, D)))
    sh["gfin_bc"] = f(np.broadcast_to(inp["norm_final"][None, :], (128, D)))
    win3 = w_in.reshape(NDC, 128, 5120)
    watt = np.empty((8, 128, NDC, 384), np.float32)
    wlru = np.empty((8, 128, NDC, 256), np.float32)
    for i in range(8):
        for j, base in enumerate((0, 1024, 2048)):
            watt[i, :, :, j * 128:(j + 1) * 128] = win3[:, :, base + i * 128:base + (i + 1) * 128].transpose(1, 0, 2)
        for j, base in enumerate((3072, 4096)):
            wlru[i, :, :, j * 128:(j + 1) * 128] = win3[:, :, base + i * 128:base + (i + 1) * 128].transpose(1, 0, 2)
    sh["w_att"] = watt
    sh["w_lru"] = wlru
    wabd = np.zeros((8, 128, 256), np.float32)
    wa = f(inp["w_a"])
    wx = f(inp["w_x"])
    for cc in range(8):
        for j in range(2):
            wabd[cc, j * 64:(j + 1) * 64, j * 64:(j + 1) * 64] = wa[2 * cc + j]
            wabd[cc, j * 64:(j + 1) * 64, 128 + j * 64:128 + (j + 1) * 64] = wx[2 * cc + j]
    sh["wabd"] = wabd
    sh["w_out"] = f(inp["w_out"])
    sh["w_router_t"] = f(f(inp["w_router"]).reshape(NDC, 128, NE).transpose(1, 0, 2))
    bgu = f(inp["b_gate_up"]).reshape(NE, 2048, 2)
    bt = np.empty((128, NE, 32), np.float32)
    bt[:, :, 0:16] = bgu[:, :, 0].reshape(NE, 16, 128).transpose(2, 0, 1)
    bt[:, :, 16:32] = bgu[:, :, 1].reshape(NE, 16, 128).transpose(2, 0, 1)
    sh["bgu"] = bt.reshape(128, NE * 32)
    sh["b_down"] = f(inp["b_down"])
    wgu = np.asarray(inp["w_gate_up"], dtype=np.float32).reshape(NE, NDC, 128, 8, 256, 2)
    sh["wgu_t"] = np.ascontiguousarray(wgu.transpose(0, 3, 2, 1, 5, 4)).reshape(NE, 8, 128, 8192)
    wdn = np.asarray(inp["w_down"], dtype=np.float32).reshape(NE, 16, 128, 4, 512)
    sh["wdn_t"] = np.ascontiguousarray(wdn.transpose(0, 3, 2, 1, 4)).reshape(NE, 4, 128, 8192)
    smv = np.zeros((128, NSM), np.float32)
    gm = np.concatenate([f(inp["attn_out_norm"]), f(inp["lru_out_norm"])])
    smv[:, SM_GMIXT:SM_GMIXT + 16] = gm.reshape(16, 128).T
    cw = f(inp["conv_w"])
    smv[:, SM_CONVW:SM_CONVW + 32] = cw.reshape(4, 8, 128).transpose(2, 1, 0).reshape(128, 32)
    smv[:, SM_CONVB:SM_CONVB + 8] = f(inp["conv_b"]).reshape(8, 128).T
    smv[:, SM_BA:SM_BA + 8] = f(inp["b_a"]).reshape(8, 128).T
    smv[:, SM_BX:SM_BX + 8] = f(inp["b_x"]).reshape(8, 128).T
    smv[:, SM_LAM:SM_LAM + 8] = f(inp["lru_lambda"]).reshape(8, 128).T
    smv[:, SM_BROUT:SM_BROUT + NE] = f(inp["b_router"])[None, :]
    sh["smalls"] = smv
    return sh


def core_inputs(inp, sh, c):
    b, half = c // 2, c % 2
    x = np.asarray(inp["x"], dtype=np.float32)
    if half == 1:
        xw = np.ascontiguousarray(x[b])
    else:
        xw = np.concatenate([np.zeros((OWN, D), np.float32), x[b, :OWN]], axis=0)
    m = dict(sh)
    smv = sh["smalls"].copy()
    fl = 1.0 if half == 1 else 0.0
    smv[:, SM_FLAG] = fl
    smv[0:64, SM_FLAG + 1] = fl
    smv[64:128, SM_FLAG + 1] = 1.0
    m["smalls"] = smv
    m["xw"] = xw
    return m


_NC_CACHE = {}


def kernel(**inputs):
    sh = prep_shared(inputs)
    in_maps = [core_inputs(inputs, sh, c) for c in range(8)]
    if "nc" not in _NC_CACHE:
        _NC_CACHE["nc"] = build()
    res = run_bass_kernel_spmd(_NC_CACHE["nc"], in_maps, core_ids=list(range(8)))
    out = np.empty((4, 2048, D), np.float32)
    for c in range(8):
        b, half = c // 2, c % 2
        out[b, half * OWN:(half + 1) * OWN, :] = np.asarray(res.results[c]["out"], dtype=np.float32)
    return out
```

```python
import contextlib
import numpy as np
import concourse.bass as bass
import concourse.mybir as mybir
from concourse.alu_op_type import AluOpType as ALU
from concourse.bass_utils import run_bass_kernel_spmd

F32 = mybir.dt.float32
BF16 = mybir.dt.bfloat16
AF = mybir.ActivationFunctionType

D = 2048
WIN = 2048
OWN = 1024
NDC = 16
NE = 32
CAP = 256
EPS = 1e-6
NRING = 3
KPRE = 9
ENGS = ("sync", "act", "pool", "dve", "pe")


def ssl(start, n, step):
    return slice(start, start + (n - 1) * step + 1, step)


class Buf:
    __slots__ = ("name", "lw", "rd", "excl")

    def __init__(self, name="", excl=False):
        self.name = name
        self.lw = None
        self.rd = {}
        self.excl = excl


class DSem:
    def __init__(self, h, key):
        self.h = h
        self.key = key
        self.count = 0


class Sched:
    SELF_SYNC = ("act", "pool", "dve")

    def __init__(self, nc, stack):
        self.nc = nc
        self.stack = stack
        self.items = {e: [] for e in ENGS}
        self.cnt = {e: 0 for e in ENGS}
        self.waited = {e: {} for e in ENGS}
        self.semh = {}
        for e in ENGS:
            self.semh[("eng", e)] = stack.enter_context(nc.semaphore("s_" + e))
        self.ndsem = 0
        self.nops = 0

    def dsem(self):
        k = ("dma", self.ndsem)
        self.semh[k] = self.stack.enter_context(self.nc.semaphore(f"d{self.ndsem}"))
        self.ndsem += 1
        return DSem(self.semh[k], k)

    def _collect(self, eng, reads, writes):
        deps = {}

        def add(k, v):
            if deps.get(k, 0) < v:
                deps[k] = v
        for b in reads:
            if b.lw is not None:
                add(*b.lw)
        for b in writes:
            if b.lw is not None:
                add(*b.lw)
            for k, v in b.rd.items():
                add(k, v)
        out = []
        for k, v in deps.items():
            if k == ("eng", eng) and eng not in self.SELF_SYNC:
                continue
            if self.waited[eng].get(k, 0) >= v:
                continue
            self.waited[eng][k] = v
            out.append((k, v))
        return out

    def _mark(self, ev, reads, writes):
        k, v = ev
        for b in reads:
            if b.rd.get(k, 0) < v:
                b.rd[k] = v
        for b in writes:
            b.lw = ev
            b.rd = {}

    def op(self, eng, fn, reads=(), writes=()):
        ex = [b for b in reads if b.excl]
        if ex:
            writes = list(writes) + [b for b in ex if b not in writes]
            reads = [b for b in reads if not b.excl]
        waits = self._collect(eng, reads, writes)
        self.cnt[eng] += 1
        ev = (("eng", eng), self.cnt[eng])
        self.items[eng].append((waits, fn, ev, 1))
        self._mark(ev, reads, writes)
        self.nops += 1
        return ev

    def dma(self, q, out, in_, ds, reads=(), writes=(), **kw):
        waits = self._collect(q, reads, writes)
        ds.count += 16
        ev = (ds.key, ds.count)
        self.items[q].append(
            (waits, (lambda e, out=out, in_=in_, kw=kw: e.dma_start(out=out, in_=in_, **kw)), ev, 16))
        self._mark(ev, reads, writes)
        self.nops += 1
        return ev

    def alias(self, olds, new):
        for b in olds:
            if b.lw is not None:
                k, v = b.lw
                if new.rd.get(k, 0) < v:
                    new.rd[k] = v
            for k, v in b.rd.items():
                if new.rd.get(k, 0) < v:
                    new.rd[k] = v

    def final_wait(self, eng, evs):
        waits = []
        for k, v in evs:
            if self.waited[eng].get(k, 0) < v:
                self.waited[eng][k] = v
                waits.append((k, v))
        self.items[eng].append((waits, None, None, 0))

    def emit(self):
        nc = self.nc
        engobj = {"sync": "sync", "act": "scalar", "pool": "gpsimd", "dve": "vector", "pe": "tensor"}
        semh = self.semh
        with nc.Block() as block:
            for e in ENGS:
                def body(eng, items=self.items[e]):
                    for waits, fn, ev, inc in items:
                        for k, v in waits:
                            eng.wait_ge(semh[k], v)
                        if fn is not None:
                            fn(eng).then_inc(semh[ev[0]], inc)
                getattr(block, engobj[e])(body)


CST_IDENT = 0
CST_R2 = 128
CST_R16 = 640
CST_IOTA = 1664
CST_LTRI = 1920
NCST = 2048
MASKV = -1.0e6


def _make_cst():
    c = np.zeros((128, NCST), np.float32)
    c[:, CST_IDENT:CST_IDENT + 128] = np.eye(128, dtype=np.float32)
    k = np.arange(128)[:, None].astype(np.float64)
    q = np.arange(128)[None, :].astype(np.float64)
    rprev = np.where(q <= k, -(q + 128 - k), MASKV)
    rcur = np.where(q >= k, -(q - k), MASKV)
    r2 = np.concatenate([rprev, rcur, rprev, rcur], axis=1)
    c[:, CST_R2:CST_R2 + 512] = r2
    for g in range(2):
        blk = rcur[:, 64 + 32 * g: 64 + 32 * g + 32]
        c[:, CST_R16 + g * 512: CST_R16 + (g + 1) * 512] = np.tile(blk, (1, 16))
    c[:, CST_IOTA:CST_IOTA + 256] = np.arange(256, dtype=np.float32)[None, :]
    c[:, CST_LTRI:CST_LTRI + 128] = (k < q).astype(np.float32)
    return c


SM_GMIXT = 0
SM_CONVW = 16
SM_CONVB = 48
SM_BA = 56
SM_BX = 64
SM_LAM = 72
SM_BROUT = 80
SM_FLAG = 112
NSM = 128


def build(debug=None):
    nc = bass.Bass("TRN2", target_bir_lowering=False)

    def din(name, shape, dt=F32):
        return nc.dram_tensor(name, list(shape), dt, kind="ExternalInput").ap()

    xw = din("xw", [WIN, D])
    cst_d = din("cst", [128, NCST])
    sm_d = din("smalls", [128, NSM])
    gmix_d = din("gmix_bc", [128, D])
    gffn_d = din("gffn_bc", [128, D])
    gfin_d = din("gfin_bc", [128, D])
    watt_d = din("w_att", [8, 128, NDC, 384])
    wlru_d = din("w_lru", [8, 128, NDC, 256])
    wabd_d = din("wabd", [8, 128, 256])
    wout_d = din("w_out", [D, D])
    wrt_d = din("w_router_t", [128, NDC, NE])
    bgu_d = din("bgu", [128, NE * 32])
    bdn_d = din("b_down", [NE, D])
    big = debug in (None, "moe1", "x3")
    wgu_d = din("wgu_t", [NE, 8, 128, 8192]) if big else None
    wdn_d = din("wdn_t", [NE, 4, 128, 8192]) if big else None
    out_d = nc.dram_tensor("out", [OWN, D], F32, kind="ExternalOutput").ap()
    wgu_bf = nc.dram_tensor("wgu_bf", [NE, 4, 128, 8192], BF16, kind="Internal").ap() if big else None
    watt_bf = nc.dram_tensor("watt_bf", [8, 128, NDC * 384], BF16, kind="Internal").ap()
    wlru_bf = nc.dram_tensor("wlru_bf", [8, 128, NDC * 256], BF16, kind="Internal").ap()
    wo_bf = nc.dram_tensor("wo_bf", [4, 128, 8192], BF16, kind="Internal").ap()
    dbg_d = None
    if debug is not None:
        dbg_d = nc.dram_tensor("dbg", [128, 16384], F32, kind="ExternalOutput").ap()

    with contextlib.ExitStack() as st:
        S = Sched(nc, st)
        ARENA_KB = 207
        arena = st.enter_context(nc.sbuf_tensor("arena", [128, ARENA_KB * 256], F32))

        def V(off_kb, nbytes, dt=F32, pat=None, **kw):
            off = int(round(off_kb * 1024))
            assert off % 4 == 0 and nbytes % 4 == 0 and off + nbytes <= ARENA_KB * 1024, (off_kb, nbytes)
            a = arena[:, off // 4:(off + nbytes) // 4]
            if dt != F32:
                a = a.bitcast(dt)
            if pat:
                a = a.rearrange(pat, **kw)
            return a

        banks = [st.enter_context(nc.psum_tensor(f"pb{i}", [128, 512], F32)) for i in range(8)]
        PB = [Buf(f"pb{i}", excl=True) for i in range(8)]

        def bank_bf(i):
            return banks[i][:].bitcast(BF16)

        K_CST = 189
        cst = V(K_CST, NCST * 4)
        sm = V(K_CST + 8, 1024)
        identB = V(K_CST + 9, 256, BF16)
        ltriB = V(K_CST + 9.25, 256, BF16)
        onesB = V(K_CST + 9.5, 256, BF16)
        flag64 = V(K_CST + 9.75, 256, BF16)
        flag16_64 = V(K_CST + 10, 256, BF16)
        ones64 = onesB
        onesF = V(K_CST + 10.25, 512)
        bgu = V(K_CST + 11, 4096)
        small_t = V(K_CST + 15, 2048)
        CST, SM, BGU, STAT = Buf("cst"), Buf("sm"), Buf("bgu"), Buf("stat")
        identF = cst[:, CST_IDENT:CST_IDENT + 128]
        R2 = cst[:, CST_R2:CST_R2 + 512]
        R16 = [cst[:, CST_R16 + g * 512:CST_R16 + (g + 1) * 512] for g in range(2)]
        iota = cst[:, CST_IOTA:CST_IOTA + 256]
        flag = sm[:, SM_FLAG:SM_FLAG + 1]
        flag16 = sm[:, SM_FLAG + 1:SM_FLAG + 2]
        SMD = 128
        c_lru = sm[:, SMD:SMD + 8]
        c2_lru = sm[:, SMD + 8:SMD + 16]
        tmpA = sm[:, SMD + 16:SMD + 24]
        tmpB = sm[:, SMD + 24:SMD + 32]
        tmpC = sm[:, SMD + 32:SMD + 40]
        s0t = sm[:, SMD + 40:SMD + 41]

        dcst = S.dsem()
        S.dma("sync", cst, cst_d, dcst, writes=[CST])
        S.dma("sync", sm[:, 0:NSM], sm_d, dcst, writes=[SM])
        S.dma("sync", bgu, bgu_d, dcst, writes=[BGU])
        for b in (CST, SM, BGU):
            b.lw = (dcst.key, dcst.count)

        CB = Buf("constsB")
        S.op("dve", lambda e: e.tensor_copy(out=identB, in_=identF), reads=[CST], writes=[CB])
        S.op("dve", lambda e: e.tensor_copy(out=ltriB, in_=cst[:, CST_LTRI:CST_LTRI + 128]), reads=[CST], writes=[CB])
        S.op("dve", lambda e: e.memset(onesB, 1.0), writes=[CB])
        S.op("dve", lambda e: e.memset(onesF, 1.0), writes=[CB])
        S.op("dve", lambda e: e.tensor_scalar(out=flag64, in0=ones64, scalar1=flag, scalar2=None, op0=ALU.mult),
             reads=[SM, CB], writes=[CB])
        S.op("dve", lambda e: e.tensor_scalar(out=flag16_64, in0=ones64, scalar1=flag16, scalar2=None, op0=ALU.mult),
             reads=[SM, CB], writes=[CB])
        bgu3 = bgu.rearrange("p (e j) -> p e j", j=32)
        S.op("dve", lambda e: e.tensor_scalar(out=bgu3[:, :, 16:32], in0=bgu3[:, :, 16:32], scalar1=1.0, scalar2=None,
                                              op0=ALU.add), reads=[BGU], writes=[BGU])
        lam = sm[:, SM_LAM:SM_LAM + 8]
        S.op("act", lambda e: e.activation(out=tmpA, in_=lam, func=AF.Exp, scale=-1.0), reads=[SM], writes=[SM])
        S.op("dve", lambda e: e.tensor_scalar(out=tmpB, in0=tmpA, scalar1=2.0, scalar2=None, op0=ALU.add), reads=[SM], writes=[SM])
        S.op("dve", lambda e: e.reciprocal(out=tmpB, in_=tmpB), reads=[SM], writes=[SM])
        S.op("dve", lambda e: e.tensor_tensor(out=tmpA, in0=tmpA, in1=tmpB, op=ALU.mult), reads=[SM], writes=[SM])
        S.op("dve", lambda e: e.tensor_tensor(out=tmpB, in0=tmpA, in1=tmpA, op=ALU.mult), reads=[SM], writes=[SM])
        S.op("dve", lambda e: e.tensor_scalar(out=tmpC, in0=tmpB, scalar1=1.0 / 7, scalar2=1.0 / 5, op0=ALU.mult, op1=ALU.add), reads=[SM], writes=[SM])
        S.op("dve", lambda e: e.tensor_tensor(out=tmpC, in0=tmpC, in1=tmpB, op=ALU.mult), reads=[SM], writes=[SM])
        S.op("dve", lambda e: e.tensor_scalar(out=tmpC, in0=tmpC, scalar1=1.0 / 3, scalar2=None, op0=ALU.add), reads=[SM], writes=[SM])
        S.op("dve", lambda e: e.tensor_tensor(out=tmpC, in0=tmpC, in1=tmpB, op=ALU.mult), reads=[SM], writes=[SM])
        S.op("dve", lambda e: e.tensor_scalar(out=tmpC, in0=tmpC, scalar1=1.0, scalar2=None, op0=ALU.add), reads=[SM], writes=[SM])
        S.op("dve", lambda e: e.tensor_tensor(out=tmpC, in0=tmpC, in1=tmpA, op=ALU.mult), reads=[SM], writes=[SM])
        S.op("dve", lambda e: e.tensor_scalar(out=c_lru, in0=tmpC, scalar1=-16.0, scalar2=None, op0=ALU.mult), reads=[SM], writes=[SM])
        S.op("dve", lambda e: e.tensor_scalar(out=c2_lru, in0=tmpC, scalar1=-32.0, scalar2=None, op0=ALU.mult), reads=[SM], writes=[SM])

        hT = V(0, 65536, BF16, "p (c t) -> p c t", c=NDC)
        HT = [Buf(f"hT{t}") for t in range(16)]
        mixedT = V(64, 32768, BF16, "p (c t) -> p c t", c=16)
        MX = [Buf(f"mx{c}") for c in range(16)]
        wring = [V(96 + 12 * i, 12288, BF16, "p (c n) -> p c n", c=NDC) for i in range(2)]
        WR = [Buf("wr0"), Buf("wr1")]
        dwr = [S.dsem(), S.dsem()]
        TK = 120
        gmix_bc = V(168, 8192)
        GM = Buf("gmix")
        dg = S.dsem()
        S.dma("sync", gmix_bc, gmix_d, dg, writes=[GM])

        units = [("att", i) for i in range(8)] + [("lru", i) for i in range(8)]

        NPS = 4
        dpre = [S.dsem() for _ in range(NPS)]
        pre_list = [("unit", 0, ui) for ui in range(2, 16)] + [("wo", 0, n4) for n4 in range(4)]
        if big:
            for ex in range(NE):
                for pc in (1, 3, 5, 7):
                    pre_list.append(("gu", ex, pc))
        PCB = {}
        pre_evs = []
        pre_i = [0]

        def issue_precast(n):
            for _ in range(n):
                i = pre_i[0]
                if i >= len(pre_list):
                    return
                pre_i[0] += 1
                kind, ex, j = pre_list[i]
                if kind == "wo":
                    src = wout_d.rearrange("(c p) n -> p c n", p=128)[:, :, j * 512:(j + 1) * 512]
                    dst = wo_bf[j].rearrange("p (c n) -> p c n", c=NDC)
                elif kind == "unit":
                    ukind, ui_ = units[j]
                    if ukind == "att":
                        src = watt_d[ui_]
                        dst = watt_bf[ui_].rearrange("p (c n) -> p c n", c=NDC)
                    else:
                        src = wlru_d[ui_]
                        dst = wlru_bf[ui_].rearrange("p (c n) -> p c n", c=NDC)
                else:
                    src = wgu_d[ex, j].rearrange("p (a b) -> p a b", b=2048)
                    dst = wgu_bf[ex, j // 2].rearrange("p (a b) -> p a b", b=2048)
                bb = Buf(f"pre{i}")
                PCB[(kind, ex, j)] = bb
                if i >= NPS:
                    bb.rd[pre_evs[i - NPS][0]] = pre_evs[i - NPS][1]
                ev = S.dma("pool", dst, src, dpre[i % NPS], writes=[bb])
                pre_evs.append(ev)

        def issue_unit_dma(ui):
            kind, i = units[ui]
            s = ui % 2
            if ui < 2:
                if kind == "att":
                    S.dma("pool", wring[s][:, :, 0:384], watt_d[i], dwr[s], writes=[WR[s]])
                else:
                    S.dma("pool", wring[s][:, :, 0:256], wlru_d[i], dwr[s], writes=[WR[s]])
                if ui == 1:
                    issue_precast(100000)
            else:
                pb_ = PCB[("unit", 0, ui)]
                if kind == "att":
                    S.dma("sync", wring[s][:, :, 0:384], watt_bf[i].rearrange("p (c n) -> p c n", c=NDC), dwr[s], reads=[pb_], writes=[WR[s]])
                else:
                    S.dma("sync", wring[s][:, :, 0:256], wlru_bf[i].rearrange("p (c n) -> p c n", c=NDC), dwr[s], reads=[pb_], writes=[WR[s]])

        issue_unit_dma(0)

        xt = [V(TK + 8 * i, 8192) for i in range(2)]
        XT = [Buf("xt0"), Buf("xt1")]
        hb = [V(TK + 16 + 4 * i, 4096, BF16) for i in range(2)]
        HB = [Buf("hb0"), Buf("hb1")]
        sqj = V(TK + 24, 4096, BF16)
        SQJ = Buf("sqj")
        dx = [S.dsem(), S.dsem()]
        ssq = small_t[:, 0:16]
        rs = small_t[:, 16:32]
        pbi = 0
        for tc in range(16):
            s = tc % 2
            S.dma("sync", xt[s], xw[tc * 128:(tc + 1) * 128, :], dx[s], writes=[XT[s]])
            S.op("act", lambda e, s=s, tc=tc: e.activation(out=sqj, in_=xt[s], func=AF.Square, accum_out=ssq[:, tc:tc + 1]),
                 reads=[XT[s]], writes=[SQJ, STAT])
            S.op("dve", lambda e, tc=tc: e.tensor_scalar(out=rs[:, tc:tc + 1], in0=ssq[:, tc:tc + 1], scalar1=1.0 / D, scalar2=EPS,
                                                         op0=ALU.mult, op1=ALU.add), reads=[STAT], writes=[STAT])
            S.op("act", lambda e, tc=tc: e.activation(out=rs[:, tc:tc + 1], in_=rs[:, tc:tc + 1], func=AF.Sqrt), reads=[STAT], writes=[STAT])
            S.op("dve", lambda e, tc=tc: e.reciprocal(out=rs[:, tc:tc + 1], in_=rs[:, tc:tc + 1]), reads=[STAT], writes=[STAT])
            S.op("dve", lambda e, s=s, tc=tc: e.scalar_tensor_tensor(out=hb[s], in0=xt[s], scalar=rs[:, tc:tc + 1], in1=gmix_bc,
                                                                     op0=ALU.mult, op1=ALU.mult),
                 reads=[XT[s], STAT, GM], writes=[HB[s]])
            for q4 in range(4):
                k = pbi % 4
                pbi += 1
                pv = bank_bf(k)[:, 0:512].rearrange("p (j t) -> p j t", j=4)
                for j in range(4):
                    dc = q4 * 4 + j
                    S.op("pe", lambda e, pv=pv, j=j, s=s, dc=dc: e.transpose(out=pv[:, j, :], in_=hb[s][:, dc * 128:(dc + 1) * 128],
                                                                            identity=identB),
                         reads=[HB[s], CB], writes=[PB[k]])
                dst = hT[:, q4 * 4:q4 * 4 + 4, tc * 128:(tc + 1) * 128]
                if q4 % 2 == 0:
                    S.op("act", lambda e, dst=dst, pv=pv: e.activation(out=dst, in_=pv, func=AF.Copy), reads=[PB[k]], writes=[HT[tc]])
                else:
                    S.op("dve", lambda e, dst=dst, pv=pv: e.tensor_copy(out=dst, in_=pv), reads=[PB[k]], writes=[HT[tc]])

        if debug == "A":
            tmpf = V(TK + 28, 16384)
            TMPF = Buf("tmpf")
            S.op("dve", lambda e: e.tensor_copy(out=tmpf, in_=hT[:, 0:2, :].rearrange("p c t -> p (c t)")), reads=HT, writes=[TMPF])
            dd = S.dsem()
            ev = S.dma("sync", dbg_d[:, 0:4096], tmpf, dd, reads=[TMPF])
            S.final_wait("sync", [ev])
            S.emit()
            return nc
        qz = [[V(TK + 0 + 4 * bs + 2 * hh, 2048, BF16) for hh in range(2)] for bs in range(2)]
        QZ = [[Buf(f"qz{bs}{hh}") for hh in range(2)] for bs in range(2)]
        kTt = [V(TK + 8 + 4 * bs, 4096, BF16) for bs in range(2)]
        KT = [Buf("kT0"), Buf("kT1")]
        NVB = 37
        vt = [V(TK + 16 + 9.5 * bs, NVB * 256, BF16, "p (b n) -> p b n", b=NVB) for bs in range(2)]
        VB = [[Buf(f"v{bs}_{i}") for i in range(NVB)] for bs in range(2)]
        sbt = [V(TK + 35 + 2 * i, 2048) for i in range(2)]
        SBT = [Buf("sb0"), Buf("sb1")]
        pTt = [V(TK + 39 + i, 1024, BF16) for i in range(2)]
        PT = [Buf("pT0"), Buf("pT1")]
        rden = [V(TK + 41 + 2 * i, 2048) for i in range(2)]
        RD = [Buf("rd0"), Buf("rd1")]
        stageA_bufs = XT + HB + [SQJ]
        for bs in range(2):
            for hh in range(2):
                S.alias(stageA_bufs, QZ[bs][hh])
            S.alias(stageA_bufs, KT[bs])
            for b_ in VB[bs]:
                S.alias(stageA_bufs, b_)
        for b_ in SBT + PT + RD:
            S.alias(stageA_bufs, b_)
        if debug == "B01":
            S.op("pool", lambda e: e.memset(mixedT[:, 0, :], 7.0), writes=[MX[0]])
            S.op("pool", lambda e: e.memset(rden[0], 5.0), writes=[RD[0]])
        for bs in range(2):
            for hh in range(2):
                S.op("dve", lambda e, bs=bs, hh=hh: e.memset(qz[bs][hh], 0.0), writes=[QZ[bs][hh]])

        vblocks = []
        v1idx, v4idx, v16idx = {}, {}, {}
        for n in range(7, 16):
            v1idx[n] = len(vblocks)
            vblocks.append((slice(n * 128, (n + 1) * 128), 1 if n == 7 else 0))
        for r4 in range(4):
            for n4 in (1, 2, 3):
                v4idx[(r4, n4)] = len(vblocks)
                vblocks.append((ssl(r4 + 512 * n4, 128, 4), 1 if n4 == 1 else 0))
        for r in range(16):
            v16idx[r] = len(vblocks)
            vblocks.append((ssl(r, 128, 16), 2))
        assert len(vblocks) == NVB
        ALLHT = HT

        def ht_bufs(sl):
            if sl.step is None or sl.step == 1:
                return HT[sl.start // 128:(sl.stop + 127) // 128]
            return ALLHT

        accbank = [0]

        def next_acc():
            k = accbank[0] % 2
            accbank[0] += 1
            return k

        scb = [0]
        evtoggle = [0]

        def evac_copy(dst, src, reads, writes, scale=None):
            evtoggle[0] += 1
            if evtoggle[0] % 2 == 0:
                if scale is None:
                    S.op("act", lambda e: e.activation(out=dst, in_=src, func=AF.Copy), reads=reads, writes=writes)
                else:
                    S.op("act", lambda e: e.activation(out=dst, in_=src, func=AF.Copy, scale=scale), reads=reads, writes=writes)
            else:
                if scale is None:
                    S.op("dve", lambda e: e.tensor_copy(out=dst, in_=src), reads=reads, writes=writes)
                else:
                    S.op("dve", lambda e: e.tensor_scalar(out=dst, in0=src, scalar1=scale, scalar2=None, op0=ALU.mult),
                         reads=reads, writes=writes)

        NUMB = [PB[4], PB[5]]
        DENB = [PB[6], PB[7]]
        pvstep = [0]
        NUMK = [4, 5]
        DENK = [6, 7]

        def inproj_steps(ui, hp):
            ws = ui % 2
            bs = hp % 2
            w = wring[ws]
            for g in range(2):
                k = next_acc()
                for dc in range(NDC):
                    S.op("pe", lambda e, k=k, dc=dc, g=g: e.matmul(banks[k][:], lhsT=w[:, dc, 0:128],
                                                                   rhs=hT[:, dc, OWN + g * 512:OWN + (g + 1) * 512],
                                                                   start=(dc == 0), stop=(dc == NDC - 1)),
                         reads=[WR[ws]] + HT[8 + 4 * g:12 + 4 * g], writes=[PB[k]])
                S.op("act", lambda e, k=k, g=g: e.activation(out=qz[bs][0][0:64, g * 512:(g + 1) * 512], in_=banks[k][0:64, :],
                                                             func=AF.Copy, scale=0.125), reads=[PB[k]], writes=[QZ[bs][0]])
                S.op("act", lambda e, k=k, g=g: e.activation(out=qz[bs][1][64:128, g * 512:(g + 1) * 512], in_=banks[k][64:128, :],
                                                             func=AF.Copy, scale=0.125), reads=[PB[k]], writes=[QZ[bs][1]])
                yield
            for g in range(4):
                k = next_acc()
                for dc in range(NDC):
                    S.op("pe", lambda e, k=k, dc=dc, g=g: e.matmul(banks[k][:], lhsT=w[:, dc, 128:256],
                                                                   rhs=hT[:, dc, g * 512:(g + 1) * 512],
                                                                   start=(dc == 0), stop=(dc == NDC - 1)),
                         reads=[WR[ws]] + HT[4 * g:4 * g + 4], writes=[PB[k]])
                S.op("act", lambda e, k=k, g=g: e.activation(out=kTt[bs][:, g * 512:(g + 1) * 512], in_=banks[k][:], func=AF.Copy),
                     reads=[PB[k]], writes=[KT[bs]])
                yield
            for b0 in range(0, NVB, 4):
                k = next_acc()
                nb = min(4, NVB - b0)
                for j in range(nb):
                    sl, fk = vblocks[b0 + j]
                    for dc in range(NDC):
                        S.op("pe", lambda e, k=k, j=j, sl=sl, dc=dc: e.matmul(banks[k][:, j * 128:(j + 1) * 128], lhsT=hT[:, dc, sl],
                                                                              rhs=w[:, dc, 256:384],
                                                                              start=(dc == 0), stop=(dc == NDC - 1)),
                             reads=[WR[ws]] + ht_bufs(sl), writes=[PB[k]])
                for j in range(nb):
                    sl, fk = vblocks[b0 + j]
                    src = banks[k][:, j * 128:(j + 1) * 128]
                    dst = vt[bs][:, b0 + j, :]
                    if fk == 0:
                        S.op("act", lambda e, dst=dst, src=src: e.activation(out=dst, in_=src, func=AF.Copy),
                             reads=[PB[k]], writes=[VB[bs][b0 + j]])
                    else:
                        sc = flag if fk == 1 else flag16
                        S.op("act", lambda e, dst=dst, src=src, sc=sc: e.activation(out=dst, in_=src, func=AF.Copy, scale=sc),
                             reads=[PB[k], SM], writes=[VB[bs][b0 + j]])
                yield
            if ui + 2 < len(units):
                issue_unit_dma(ui + 2)

        def attention_steps(ui, hp):
            bs = hp % 2
            kT = kTt[bs]
            flat = []
            for hh in (range(2) if debug not in ("B00", "B01", "B0a") else ((0,) if debug in ("B00", "B0a") else (1,))):
                h = 2 * hp + hh
                slope = 2.0 ** (-8.0 * (h + 1) / 16.0)
                for g in range(2 if debug not in ("B00", "B01") else 1):
                    items = []
                    for half in range(2):
                        mm, pv = [], []
                        for qi in range(2):
                            n = 8 + 4 * g + 2 * half + qi
                            qsl = slice((n - 8) * 128, (n - 7) * 128)
                            ocol = slice((n - 8 - 4 * g) * 128, (n - 7 - 4 * g) * 128)
                            for kb in range(2):
                                nk = n - 1 + kb
                                col = (qi * 2 + kb) * 128
                                mm.append((col, 128, slice(nk * 128, (nk + 1) * 128), qsl))
                                pv.append((col, 128, v1idx[nk], flag64 if nk == 7 else ones64, ocol))
                        items.append((mm, R2, slope * 1.0, pv))
                    for cp in range(2):
                        mm, pv = [], []
                        for ci in range(2):
                            r4 = 2 * cp + ci
                            n4 = 2 + g
                            qsl = ssl(r4 + 512 * g, 128, 4)
                            ocol = ssl(r4, 128, 4)
                            for kb in range(2):
                                nk = n4 - 1 + kb
                                col = (ci * 2 + kb) * 128
                                mm.append((col, 128, ssl(r4 + 512 * nk, 128, 4), qsl))
                                pv.append((col, 128, v4idx[(r4, nk)], flag64 if nk == 1 else ones64, ocol))
                        items.append((mm, R2, slope * 4.0, pv))
                    mm, pv = [], []
                    for r in range(16):
                        qsl = ssl(r + 512 * g, 32, 16)
                        mm.append((r * 32, 32, ssl(r, 128, 16), qsl))
                        pv.append((r * 32, 32, v16idx[r], flag16_64, ssl(r, 32, 16)))
                    items.append((mm, R16[g], slope * 16.0, pv))
                    x = pvstep[0] % 2
                    pvstep[0] += 1
                    for ii, (mm, Rt, scal, pv) in enumerate(items):
                        flat.append(dict(hh=hh, g=g, x=x, mm=mm, Rt=Rt, scal=scal, pv=pv, first=(ii == 0), last=(ii == len(items) - 1)))
            for it in flat:
                it["sk"] = 2 + (scb[0] % 2)
                it["ti"] = scb[0] % 2
                scb[0] += 1

            def emit_scores(it):
                q = qz[bs][it["hh"]]
                sk = it["sk"]
                for col, wdt, ksl, qsl in it["mm"]:
                    S.op("pe", lambda e, sk=sk, col=col, wdt=wdt, ksl=ksl, qsl=qsl, q=q: e.matmul(
                        banks[sk][:, col:col + wdt], lhsT=kT[:, ksl], rhs=q[:, qsl], start=True, stop=True,
                        skip_group_check=True), reads=[KT[bs], QZ[bs][it["hh"]]], writes=[PB[sk]])

            def emit_rest(it):
                sk, ti, x, g = it["sk"], it["ti"], it["x"], it["g"]
                hbp = 64 * it["hh"]
                Rt, scal = it["Rt"], it["scal"]
                S.op("dve", lambda e, sk=sk, ti=ti, Rt=Rt, scal=scal: e.scalar_tensor_tensor(
                    out=sbt[ti], in0=Rt, scalar=float(scal), in1=banks[sk][:], op0=ALU.mult, op1=ALU.add),
                    reads=[PB[sk], CST], writes=[SBT[ti]])
                S.op("act", lambda e, ti=ti: e.activation(out=pTt[ti], in_=sbt[ti], func=AF.Exp),
                     reads=[SBT[ti]], writes=[PT[ti]])
                first = it["first"]
                for col, wdt, vi, denl, ocol in it["pv"]:
                    S.op("pe", lambda e, col=col, wdt=wdt, vi=vi, ocol=ocol, ti=ti, x=x, first=first: e.matmul(
                        banks[NUMK[x]][:, ocol], lhsT=vt[bs][:, vi, :], rhs=pTt[ti][:, col:col + wdt],
                        start=first, stop=True, skip_group_check=True),
                        reads=[VB[bs][vi], PT[ti]], writes=[NUMB[x]])
                    S.op("pe", lambda e, col=col, wdt=wdt, denl=denl, ocol=ocol, ti=ti, x=x, first=first: e.matmul(
                        banks[DENK[x]][:, ocol], lhsT=denl, rhs=pTt[ti][:, col:col + wdt],
                        start=first, stop=True, skip_group_check=True),
                        reads=[CB, PT[ti]], writes=[DENB[x]])
                    first = False
                if it["last"]:
                    ri = x
                    S.op("dve", lambda e, x=x, ri=ri, hbp=hbp: e.reciprocal(out=rden[ri][hbp:hbp + 64, :],
                                                                            in_=banks[DENK[x]][hbp:hbp + 64, :]),
                         reads=[DENB[x]], writes=[RD[ri]])
                    S.op("dve", lambda e, g=g, x=x, ri=ri, hbp=hbp: e.tensor_tensor(
                        out=mixedT[hbp:hbp + 64, hp, g * 512:(g + 1) * 512], in0=banks[NUMK[x]][hbp:hbp + 64, :],
                        in1=rden[ri][hbp:hbp + 64, :], op=ALU.mult), reads=[NUMB[x], RD[ri]], writes=[MX[hp]])

            if flat:
                emit_scores(flat[0])
            for i, it in enumerate(flat):
                if i + 1 < len(flat):
                    emit_scores(flat[i + 1])
                emit_rest(it)
                yield

        def run_all(gen):
            for _ in gen:
                pass

        def interleave(ga, gb, ratio=1):
            da = db = False
            while not (da and db):
                if not da:
                    try:
                        next(ga)
                    except StopIteration:
                        da = True
                for _ in range(ratio):
                    if not db:
                        try:
                            next(gb)
                        except StopIteration:
                            db = True

        n_att = 8 if debug not in ("B0", "B00", "B01", "B0a", "B0x", "B0y") else 1
        issue_unit_dma(1)
        run_all(inproj_steps(0, 0))
        for ui in range(n_att):
            if ui + 1 < n_att:
                interleave(attention_steps(ui, ui), inproj_steps(ui + 1, ui + 1))
            else:
                run_all(attention_steps(ui, ui))

        def dump(ap_list, evs_reads):
            dd = S.dsem()
            evs = []
            for ap, off, n in ap_list:
                evs.append(S.dma("sync", dbg_d[:, off:off + n], ap, dd, reads=evs_reads))
            S.final_wait("sync", evs)
            S.emit()

        if debug in ("B0", "B00", "B01", "B0a", "B0x", "B0y"):
            tmpf = V(0, 32768)
            TMPF = Buf("tmpf")
            S.alias(HT, TMPF)
            S.op("dve", lambda e: e.tensor_copy(out=tmpf[:, 0:1024], in_=qz[0][0]), reads=[QZ[0][0]], writes=[TMPF])
            S.op("dve", lambda e: e.tensor_copy(out=tmpf[:, 1024:2048], in_=qz[0][1]), reads=[QZ[0][1]], writes=[TMPF])
            S.op("dve", lambda e: e.tensor_copy(out=tmpf[:, 2048:4096], in_=kTt[0]), reads=[KT[0]], writes=[TMPF])
            S.op("dve", lambda e: e.tensor_copy(out=tmpf[:, 4096:4096 + 512], in_=vt[0][:, 0:4, :].rearrange("p b n -> p (b n)")), reads=VB[0], writes=[TMPF])
            S.op("dve", lambda e: e.tensor_copy(out=tmpf[:, 4608:5120], in_=banks[4][:]), reads=[NUMB[0]], writes=[TMPF])
            S.op("dve", lambda e: e.tensor_copy(out=tmpf[:, 5120:5632], in_=banks[6][:]), reads=[DENB[0]], writes=[TMPF])
            S.op("dve", lambda e: e.tensor_copy(out=tmpf[:, 5632:6144], in_=pTt[0]), reads=[PT[0]], writes=[TMPF])
            S.op("dve", lambda e: e.tensor_copy(out=tmpf[:, 6144:6656], in_=rden[0]), reads=[RD[0]], writes=[TMPF])
            S.op("dve", lambda e: e.tensor_copy(out=tmpf[:, 6656:7680], in_=mixedT[:, 0, :]), reads=[MX[0]], writes=[TMPF])
            S.op("dve", lambda e: e.tensor_copy(out=tmpf[:, 7680:8192], in_=sbt[0]), reads=[SBT[0]], writes=[TMPF])
            dump([(tmpf, 0, 8192)], [TMPF])
            return nc
        if debug == "attn":
            tmpf = V(TK, 32768)
            TMPF = Buf("tmpf")
            S.alias(stageA_bufs + [b for bs in range(2) for b in VB[bs]] + KT + SBT + PT + RD + [QZ[a][b] for a in range(2) for b in range(2)], TMPF)
            S.op("dve", lambda e: e.tensor_copy(out=tmpf, in_=mixedT[:, 0:8, :].rearrange("p c t -> p (c t)")), reads=MX[0:8], writes=[TMPF])
            dump([(tmpf, 0, 8192)], [TMPF])
            return nc

        stageB_bufs = [b for bs in range(2) for b in VB[bs]] + KT + SBT + PT + RD + [QZ[a][b] for a in range(2) for b in range(2)]
        xraw2 = [V(TK + 8.25 * i, 8448) for i in range(2)]
        gg2 = [V(TK + 16.5 + 4 * i, 4096) for i in range(2)]
        xc = V(TK + 24.5, 8192)
        ra = V(TK + 32.5, 4096)
        ix = V(TK + 36.5, 4096)
        at = V(TK + 40.5, 4096)
        tt = V(TK + 44.5, 4096)
        hh_ = V(TK + 48.5, 4096)
        xs = V(TK + 52.5, 2048)
        uu = V(TK + 54.5, 2048)
        wabd = [V(TK + 56.5 + i, 1024) for i in range(2)]
        XC, RA, IX, AT, TT, HH, XS, UU = (Buf(n) for n in ("xc", "ra", "ix", "at", "tt", "hh", "xs", "uu"))
        XRAW2 = [Buf("xraw0"), Buf("xraw1")]
        GG2 = [Buf("gg0"), Buf("gg1")]
        WAB = [Buf("wab0"), Buf("wab1")]
        dwab = [S.dsem(), S.dsem()]
        stageC_list = [XC, RA, IX, AT, TT, HH, XS, UU] + XRAW2 + GG2 + WAB
        for b_ in stageC_list:
            S.alias(stageA_bufs + stageB_bufs + [GM], b_)
        for i in range(2):
            S.op("dve", lambda e, i=i: e.memset(xraw2[i][:, 0:3], 0.0), writes=[XRAW2[i]])

        def lru_inproj_steps(ui, cc):
            ws = ui % 2
            w = wring[ws]
            xraw = xraw2[cc % 2]
            XRAW = XRAW2[cc % 2]
            gg = gg2[cc % 2]
            GG = GG2[cc % 2]
            S.dma("sync", wabd[cc % 2], wabd_d[cc], dwab[cc % 2], writes=[WAB[cc % 2]])
            for g in range(4):
                k = next_acc()
                for dc in range(NDC):
                    S.op("pe", lambda e, k=k, dc=dc, g=g: e.matmul(banks[k][:], lhsT=w[:, dc, 0:128], rhs=hT[:, dc, g * 512:(g + 1) * 512],
                                                                   start=(dc == 0), stop=(dc == NDC - 1)),
                         reads=[WR[ws]] + HT[4 * g:4 * g + 4], writes=[PB[k]])
                S.op("act", lambda e, k=k, g=g: e.activation(out=xraw[:, 3 + g * 512:3 + (g + 1) * 512], in_=banks[k][:], func=AF.Copy),
                     reads=[PB[k]], writes=[XRAW])
                yield
            for g in range(2):
                k = next_acc()
                for dc in range(NDC):
                    S.op("pe", lambda e, k=k, dc=dc, g=g: e.matmul(banks[k][:], lhsT=w[:, dc, 128:256],
                                                                   rhs=hT[:, dc, OWN + g * 512:OWN + (g + 1) * 512],
                                                                   start=(dc == 0), stop=(dc == NDC - 1)),
                         reads=[WR[ws]] + HT[8 + 4 * g:12 + 4 * g], writes=[PB[k]])
                S.op("act", lambda e, k=k: e.activation(out=xs, in_=banks[k][:], func=AF.Copy), reads=[PB[k]], writes=[XS])
                S.op("act", lambda e, k=k: e.activation(out=uu, in_=banks[k][:], func=AF.Square), reads=[PB[k]], writes=[UU])
                S.op("dve", lambda e: e.tensor_scalar(out=uu, in0=uu, scalar1=0.044715, scalar2=1.0, op0=ALU.mult, op1=ALU.add),
                     reads=[UU], writes=[UU])
                S.op("dve", lambda e: e.tensor_tensor(out=uu, in0=uu, in1=xs, op=ALU.mult), reads=[UU, XS], writes=[UU])
                S.op("act", lambda e: e.activation(out=uu, in_=uu, func=AF.Sigmoid, scale=1.5957691216057308), reads=[UU], writes=[UU])
                S.op("dve", lambda e, g=g: e.tensor_tensor(out=gg[:, g * 512:(g + 1) * 512], in0=uu, in1=xs, op=ALU.mult),
                     reads=[UU, XS], writes=[GG])
                yield
            if ui + 2 < len(units):
                issue_unit_dma(ui + 2)

        def lru_chain_steps(ui, cc):
            xraw = xraw2[cc % 2]
            XRAW = XRAW2[cc % 2]
            gg = gg2[cc % 2]
            GG = GG2[cc % 2]
            wa = wabd[cc % 2]
            cw = lambda i: sm[:, SM_CONVW + cc * 4 + i:SM_CONVW + cc * 4 + i + 1]
            S.op("dve", lambda e: e.tensor_scalar(out=xc, in0=xraw[:, 0:WIN], scalar1=cw(0), scalar2=sm[:, SM_CONVB + cc:SM_CONVB + cc + 1],
                                                  op0=ALU.mult, op1=ALU.add), reads=[XRAW, SM], writes=[XC])
            for i in range(1, 4):
                S.op("dve", lambda e, i=i: e.scalar_tensor_tensor(out=xc, in0=xraw[:, i:i + WIN], scalar=cw(i), in1=xc,
                                                                  op0=ALU.mult, op1=ALU.add), reads=[XRAW, SM, XC], writes=[XC])
            yield
            for hf in range(2):
                for g2 in range(2):
                    t0 = hf * 1024 + g2 * 512
                    for which, dstb, DB, bcol in ((0, ra, RA, SM_BA), (1, ix, IX, SM_BX)):
                        k = next_acc()
                        S.op("pe", lambda e, k=k, which=which, t0=t0: e.matmul(banks[k][:], lhsT=wa[:, which * 128:(which + 1) * 128],
                                                                               rhs=xc[:, t0:t0 + 512], start=True, stop=True),
                             reads=[WAB[cc % 2], XC], writes=[PB[k]])
                        S.op("act", lambda e, k=k, dstb=dstb, g2=g2, bcol=bcol: e.activation(
                            out=dstb[:, g2 * 512:(g2 + 1) * 512], in_=banks[k][:], func=AF.Sigmoid,
                            bias=sm[:, bcol + cc:bcol + cc + 1]), reads=[PB[k], SM], writes=[DB])
                    yield
                S.op("act", lambda e: e.activation(out=at, in_=ra, func=AF.Exp, scale=c_lru[:, cc:cc + 1]), reads=[RA, SM], writes=[AT])
                S.op("act", lambda e: e.activation(out=tt, in_=ra, func=AF.Exp, scale=c2_lru[:, cc:cc + 1]), reads=[RA, SM], writes=[TT])
                S.op("act", lambda e: e.activation(out=tt, in_=tt, func=AF.Sqrt, scale=-1.0, bias=1.0), reads=[TT], writes=[TT])
                S.op("dve", lambda e: e.tensor_tensor(out=tt, in0=tt, in1=ix, op=ALU.mult), reads=[TT, IX], writes=[TT])
                S.op("dve", lambda e, hf=hf: e.tensor_tensor(out=tt, in0=tt, in1=xc[:, hf * 1024:(hf + 1) * 1024], op=ALU.mult),
                     reads=[TT, XC], writes=[TT])
                if hf == 0:
                    S.op("dve", lambda e: e.tensor_tensor_scan(out=hh_, data0=at, data1=tt, initial=0.0, op0=ALU.mult, op1=ALU.add),
                         reads=[AT, TT], writes=[HH])
                    S.op("dve", lambda e: e.tensor_tensor(out=s0t, in0=hh_[:, 1023:1024], in1=flag, op=ALU.mult),
                         reads=[HH, SM], writes=[SM])
                else:
                    S.op("dve", lambda e: e.tensor_tensor_scan(out=hh_, data0=at, data1=tt, initial=s0t, op0=ALU.mult, op1=ALU.add),
                         reads=[AT, TT, SM], writes=[HH])
                    S.op("dve", lambda e: e.tensor_tensor(out=mixedT[:, 8 + cc, :], in0=hh_, in1=gg, op=ALU.mult),
                         reads=[HH, GG], writes=[MX[8 + cc]])
                yield

        run_all(lru_inproj_steps(8, 0))
        for cc in range(8):
            if cc + 1 < 8:
                interleave(lru_chain_steps(8 + cc, cc), lru_inproj_steps(9 + cc, cc + 1))
            else:
                run_all(lru_chain_steps(8 + cc, cc))

        if debug == "mixed":
            tmpf = V(0, 65536)
            TMPF = Buf("tmpf")
            S.alias(HT + WR, TMPF)
            S.op("dve", lambda e: e.tensor_copy(out=tmpf, in_=mixedT.rearrange("p c t -> p (c t)")), reads=MX, writes=[TMPF])
            dump([(tmpf, 0, 16384)], [TMPF])
            return nc

        stageC_bufs = stageC_list
        p1_T = stageA_bufs + stageB_bufs + stageC_bufs
        x2 = V(0, 65536, F32, "p (c d) -> p c d", c=8)
        X2 = [Buf(f"x2_{t}") for t in range(8)]
        for b_ in X2:
            S.alias(HT, b_)
        dx2 = S.dsem()
        for t in range(8):
            S.dma("sync", x2[:, t, :], xw[OWN + t * 128:OWN + (t + 1) * 128, :], dx2, writes=[X2[t]])
        for b_ in X2:
            b_.lw = (dx2.key, dx2.count)
        wo = [V(96 + 16 * i, 16384, BF16, "p (c n) -> p c n", c=NDC) for i in range(2)]
        WO = [Buf("wo0"), Buf("wo1")]
        dwo = [S.dsem(), S.dsem()]
        for b_ in WO:
            S.alias(WR + p1_T, b_)
        woutv = wout_d.rearrange("(c p) n -> p c n", p=128)

        def issue_wo(n4):
            S.dma("sync", wo[n4 % 2].rearrange("p c n -> p (c n)"), wo_bf[n4], dwo[n4 % 2], reads=[PCB[("wo", 0, n4)]],
                  writes=[WO[n4 % 2]])
        issue_wo(0)
        sqt = [V(128 + 2 * i, 2048) for i in range(2)]
        SQT = [Buf("sqt0"), Buf("sqt1")]
        rstd_bc = V(132, 8192, F32, "p (g t) -> p g t", g=2)
        RSTD = [Buf("rstd0"), Buf("rstd1")]
        for b_ in SQT + RSTD:
            S.alias(p1_T, b_)
        for grp in range(2):
            for g in range(2):
                k = grp * 2 + g
                for ci in range(8):
                    c = grp * 8 + ci
                    ti = ci % 2
                    S.op("act", lambda e, c=c, g=g, ti=ti: e.activation(out=sqt[ti], in_=mixedT[:, c, g * 512:(g + 1) * 512], func=AF.Square),
                         reads=[MX[c]], writes=[SQT[ti]])
                    S.op("pe", lambda e, k=k, ti=ti, ci=ci: e.matmul(banks[k][:], lhsT=onesF, rhs=sqt[ti], start=(ci == 0), stop=(ci == 7)),
                         reads=[SQT[ti], CB], writes=[PB[k]])
                dst = rstd_bc[:, grp, g * 512:(g + 1) * 512]
                S.op("dve", lambda e, k=k, dst=dst: e.tensor_scalar(out=dst, in0=banks[k][:], scalar1=1.0 / 1024, scalar2=EPS,
                                                                    op0=ALU.mult, op1=ALU.add), reads=[PB[k]], writes=[RSTD[grp]])
                S.op("act", lambda e, dst=dst: e.activation(out=dst, in_=dst, func=AF.Sqrt), reads=[RSTD[grp]], writes=[RSTD[grp]])
                S.op("dve", lambda e, dst=dst: e.reciprocal(out=dst, in_=dst), reads=[RSTD[grp]], writes=[RSTD[grp]])
        for c in range(16):
            grp = c // 8
            S.op("dve", lambda e, c=c, grp=grp: e.scalar_tensor_tensor(out=mixedT[:, c, :], in0=mixedT[:, c, :],
                                                                       scalar=sm[:, SM_GMIXT + c:SM_GMIXT + c + 1],
                                                                       in1=rstd_bc[:, grp, :], op0=ALU.mult, op1=ALU.mult),
                 reads=[MX[c], SM, RSTD[grp]], writes=[MX[c]])
        opb = [0]
        for n4 in range(4):
            if n4 + 1 < 4:
                issue_wo(n4 + 1)
            s = n4 % 2
            for t in range(8):
                k = 4 + (opb[0] % 4)
                opb[0] += 1
                for kc in range(16):
                    S.op("pe", lambda e, k=k, kc=kc, t=t, s=s: e.matmul(banks[k][:], lhsT=mixedT[:, kc, t * 128:(t + 1) * 128],
                                                                        rhs=wo[s][:, kc, :], start=(kc == 0), stop=(kc == 15)),
                         reads=[MX[kc], WO[s]], writes=[PB[k]])
                S.op("dve", lambda e, k=k, t=t, n4=n4: e.tensor_tensor(out=x2[:, t, n4 * 512:(n4 + 1) * 512], in0=banks[k][:],
                                                                       in1=x2[:, t, n4 * 512:(n4 + 1) * 512], op=ALU.add),
                     reads=[PB[k], X2[t]], writes=[X2[t]])

        if debug == "x2":
            dump([(x2.rearrange("p c d -> p (c d)"), 0, 16384)], X2)
            return nc

        x2nb = V(64, 32768, BF16, "p (c d) -> p c d", c=8)
        X2N = [Buf(f"x2n{t}") for t in range(8)]
        for b_ in X2N:
            S.alias(MX, b_)
        gbc = V(175, 8192)
        GBC = Buf("gbc")
        S.alias([GM] + p1_T, GBC)
        dgb = S.dsem()
        S.dma("sync", gbc, gffn_d, dgb, writes=[GBC])
        x2nf = V(128, 8192)
        X2NF = Buf("x2nf")
        xTc = V(136, 8192, F32, "p (c t) -> p c t", c=NDC)
        XTC = Buf("xTc")
        wrt = V(144, NDC * NE * 4, F32, "p (c n) -> p c n", c=NDC)
        WRT = Buf("wrt")
        S.alias(SQT + RSTD + p1_T, X2NF)
        S.alias(SQT + RSTD + p1_T, XTC)
        S.alias(SQT + RSTD + p1_T + WO, WRT)
        dwrt = S.dsem()
        S.dma("sync", wrt, wrt_d, dwrt, writes=[WRT])
        posf = V(146, 1024, F32, "p (t e) -> p t e", t=8)
        maskf = V(147, 1024, F32, "p (t e) -> p t e", t=8)
        gatef = V(148, 1024, F32, "p (t e) -> p t e", t=8)
        gHL = V(149, 1024, BF16, "p (t e two) -> p t e two", t=8, two=2)
        maskb = V(150, 512, BF16, "p (t e) -> p t e", t=8)
        logit = V(150.5, 128)
        mx8 = V(150.625, 32)
        exq = V(150.75, 128)
        misc = V(150.875, 64)
        GTp = V(151, 4096)
        bdp = V(159, 8192)
        ROUT = [Buf(f"rout{t}") for t in range(8)]
        RTMP = Buf("rtmp")
        GTB, BDP = Buf("gtp"), Buf("bdp")
        for b_ in ROUT + [RTMP, GTB, BDP]:
            S.alias(SQT + RSTD + p1_T + WO, b_)
        S.op("dve", lambda e: e.memset(GTp, 0.0), writes=[GTB])
        S.op("dve", lambda e: e.memset(bdp, 0.0), writes=[BDP])
        dbd = S.dsem()
        S.dma("sync", bdp[0:NE, :], bdn_d, dbd, reads=[], writes=[BDP])
        ssq2 = small_t[:, 32:40]
        rs2 = small_t[:, 40:48]
        tpb = [0]
        for t in range(8):
            S.op("act", lambda e, t=t: e.activation(out=x2nf, in_=x2[:, t, :], func=AF.Square, accum_out=ssq2[:, t:t + 1]),
                 reads=[X2[t]], writes=[X2NF, STAT])
            S.op("dve", lambda e, t=t: e.tensor_scalar(out=rs2[:, t:t + 1], in0=ssq2[:, t:t + 1], scalar1=1.0 / D, scalar2=EPS,
                                                       op0=ALU.mult, op1=ALU.add), reads=[STAT], writes=[STAT])
            S.op("act", lambda e, t=t: e.activation(out=rs2[:, t:t + 1], in_=rs2[:, t:t + 1], func=AF.Sqrt), reads=[STAT], writes=[STAT])
            S.op("dve", lambda e, t=t: e.reciprocal(out=rs2[:, t:t + 1], in_=rs2[:, t:t + 1]), reads=[STAT], writes=[STAT])
            S.op("dve", lambda e, t=t: e.scalar_tensor_tensor(out=x2nf, in0=x2[:, t, :], scalar=rs2[:, t:t + 1], in1=gbc,
                                                              op0=ALU.mult, op1=ALU.mult), reads=[X2[t], STAT, GBC, X2NF], writes=[X2NF])
            S.op("act", lambda e, t=t: e.activation(out=x2nb[:, t, :], in_=x2nf, func=AF.Copy), reads=[X2NF], writes=[X2N[t]])
            for q4 in range(4):
                k = tpb[0] % 4
                tpb[0] += 1
                for j in range(4):
                    dc = q4 * 4 + j
                    S.op("pe", lambda e, k=k, j=j, dc=dc: e.transpose(out=banks[k][:, j * 128:(j + 1) * 128],
                                                                      in_=x2nf[:, dc * 128:(dc + 1) * 128], identity=identF),
                         reads=[X2NF, CST], writes=[PB[k]])
                evac_copy(xTc[:, q4 * 4:q4 * 4 + 4, :], banks[k][:].rearrange("p (j t) -> p j t", j=4), [PB[k]], [XTC])
            for dc in range(NDC):
                S.op("pe", lambda e, dc=dc: e.matmul(banks[4][:, 0:NE], lhsT=xTc[:, dc, :], rhs=wrt[:, dc, :],
                                                     start=(dc == 0), stop=(dc == NDC - 1)), reads=[XTC, WRT], writes=[PB[4]])
            S.op("dve", lambda e: e.tensor_tensor(out=logit, in0=banks[4][:, 0:NE], in1=sm[:, SM_BROUT:SM_BROUT + NE], op=ALU.add),
                 reads=[PB[4], SM], writes=[RTMP])
            S.op("dve", lambda e: e.max(out=mx8, in_=logit), reads=[RTMP], writes=[RTMP])
            S.op("dve", lambda e, t=t: e.tensor_scalar(out=maskf[:, t, :], in0=logit, scalar1=mx8[:, 3:4], scalar2=None, op0=ALU.is_ge),
                 reads=[RTMP], writes=[ROUT[t]])
            S.op("dve", lambda e: e.tensor_scalar(out=misc[:, 0:1], in0=mx8[:, 0:1], scalar1=-1.0, scalar2=None, op0=ALU.mult),
                 reads=[RTMP], writes=[RTMP])
            S.op("act", lambda e: e.activation(out=exq, in_=logit, func=AF.Exp, bias=misc[:, 0:1]), reads=[RTMP], writes=[RTMP])
            S.op("dve", lambda e, t=t: e.tensor_tensor(out=exq, in0=exq, in1=maskf[:, t, :], op=ALU.mult), reads=[RTMP, ROUT[t]], writes=[RTMP])
            S.op("dve", lambda e: e.reduce_sum(out=misc[:, 1:2], in_=exq, axis=mybir.AxisListType.X), reads=[RTMP], writes=[RTMP])
            S.op("dve", lambda e: e.reciprocal(out=misc[:, 1:2], in_=misc[:, 1:2]), reads=[RTMP], writes=[RTMP])
            S.op("dve", lambda e, t=t: e.tensor_scalar(out=gatef[:, t, :], in0=exq, scalar1=misc[:, 1:2], scalar2=None, op0=ALU.mult),
                 reads=[RTMP, ROUT[t]], writes=[ROUT[t]])
            S.op("dve", lambda e, t=t: e.tensor_copy(out=gHL[:, t, :, 0], in_=gatef[:, t, :]), reads=[ROUT[t]], writes=[ROUT[t]])
            S.op("dve", lambda e, t=t: e.tensor_tensor(out=gHL[:, t, :, 1], in0=gatef[:, t, :], in1=gHL[:, t, :, 0], op=ALU.subtract),
                 reads=[ROUT[t]], writes=[ROUT[t]])
            S.op("dve", lambda e, t=t: e.tensor_copy(out=maskb[:, t, :], in_=maskf[:, t, :]), reads=[ROUT[t]], writes=[ROUT[t]])
            for tp in range(t + 1):
                S.op("pe", lambda e, tp=tp, t=t: e.matmul(banks[5][:, 0:NE], lhsT=(onesB if tp < t else ltriB), rhs=maskb[:, tp, :],
                                                          start=(tp == 0), stop=(tp == t)),
                     reads=[ROUT[tp], CB], writes=[PB[5]])
            S.op("act", lambda e, t=t: e.activation(out=posf[:, t, :], in_=banks[5][:, 0:NE], func=AF.Copy), reads=[PB[5]], writes=[ROUT[t]])
            S.op("pe", lambda e, t=t: e.transpose(out=banks[6][0:NE, 0:128], in_=gatef[:, t, :], identity=identF),
                 reads=[ROUT[t], CST], writes=[PB[6]])
            S.op("act", lambda e, t=t: e.activation(out=GTp[0:NE, t * 128:(t + 1) * 128], in_=banks[6][0:NE, 0:128], func=AF.Copy),
                 reads=[PB[6]], writes=[GTB])
        for t in range(8):
            for dgp in range(4):
                k = 4 + (opb[0] % 4)
                opb[0] += 1
                S.op("pe", lambda e, k=k, t=t, dgp=dgp: e.matmul(banks[k][:], lhsT=GTp[:, t * 128:(t + 1) * 128],
                                                                 rhs=bdp[:, dgp * 512:(dgp + 1) * 512], start=True, stop=True),
                     reads=[GTB, BDP], writes=[PB[k]])
                S.op("dve", lambda e, k=k, t=t, dgp=dgp: e.tensor_tensor(out=x2[:, t, dgp * 512:(dgp + 1) * 512], in0=banks[k][:],
                                                                         in1=x2[:, t, dgp * 512:(dgp + 1) * 512], op=ALU.add),
                     reads=[PB[k], X2[t]], writes=[X2[t]])

        if debug == "route":
            tmpf = V(159, 16384)
            TMPF = Buf("tmpf")
            S.alias(p1_T + WO + SQT + RSTD, TMPF)
            S.op("dve", lambda e: e.tensor_copy(out=tmpf[:, 0:256], in_=gatef.rearrange("p t e -> p (t e)")), reads=ROUT, writes=[TMPF])
            S.op("dve", lambda e: e.tensor_copy(out=tmpf[:, 256:512], in_=posf.rearrange("p t e -> p (t e)")), reads=ROUT, writes=[TMPF])
            S.op("dve", lambda e: e.tensor_copy(out=tmpf[:, 512:768], in_=maskf.rearrange("p t e -> p (t e)")), reads=ROUT, writes=[TMPF])
            dump([(tmpf[:, 0:768], 0, 768)], [TMPF])
            return nc

        ring = [V(96 + 16 * i, 16384, BF16) for i in range(NRING)]
        RING = [Buf(f"ring{i}") for i in range(NRING)]
        dring = [S.dsem() for _ in range(NRING)]
        for b_ in RING:
            S.alias(WO + WR + p1_T + SQT + RSTD + [X2NF, XTC, WRT], b_)
        xg = V(151, 8192, BF16, "p (c s) -> p c s", c=NDC)
        actT = V(159, 8192, BF16, "p (c s) -> p c s", c=16)
        Sel = V(167, 4096, BF16, "p (t s) -> p t s", t=8)
        SelT = V(171, 4096, BF16, "p (b t) -> p b t", b=2)
        ysl = V(175, 8192, BF16, "p (b d) -> p b d", b=2)
        gc_t = [V(183 + i, 1024) for i in range(2)]
        sg_t = [V(185 + i, 1024) for i in range(2)]
        u1_t = [V(187 + i, 1024) for i in range(2)]
        gsl2 = [small_t[:, 64:66], small_t[:, 66:68]]
        GSLB = [Buf("gsl0"), Buf("gsl1")]
        XG, ACTT, SEL, SELT = Buf("xg"), Buf("actT"), Buf("Sel"), Buf("SelT")
        YSL = [Buf("ysl0"), Buf("ysl1"), Buf("ysl2"), Buf("ysl3")]
        EW = [Buf("ew0"), Buf("ew1")]
        for b_ in [XG, ACTT, SEL, SELT] + YSL + EW:
            S.alias(p1_T + SQT + RSTD + [X2NF, XTC, WRT, GBC, BDP, GTB], b_)
        issue_precast(1000)
        NEXP = NE if debug != "moe1" else 1
        ex_order = list(range(NE)) if NEXP == NE else [0]
        pieces = []
        for ex in ex_order:
            for pc in range(8):
                pieces.append(("gu", ex, pc))
            for dgp in range(4):
                pieces.append(("dn", ex, dgp))

        def issue_piece(pi):
            kind, ex, j = pieces[pi]
            s = pi % NRING
            if (kind, ex, j) in PCB:
                S.dma("sync", ring[s], wgu_bf[ex, j // 2], dring[s], reads=[PCB[(kind, ex, j)]], writes=[RING[s]])
            else:
                src = wgu_d[ex, j] if kind == "gu" else wdn_d[ex, j]
                S.dma("pool", ring[s].rearrange("p (a b) -> p a b", b=2048), src.rearrange("p (a b) -> p a b", b=2048),
                      dring[s], writes=[RING[s]])
        for pi in range(NRING):
            issue_piece(pi)
        ga = [0]
        gu = [0]
        dnb = [0]
        scb2 = [0]
        ewi = [0]
        pidx = [0]
        def build_sel(ex):
            for t in range(8):
                S.op("dve", lambda e, t=t, ex=ex: e.tensor_scalar(out=Sel[:, t, :], in0=iota, scalar1=posf[:, t, ex:ex + 1],
                                                                  scalar2=maskf[:, t, ex:ex + 1], op0=ALU.is_equal, op1=ALU.mult),
                     reads=[CST, ROUT[t]], writes=[SEL])

        build_sel(ex_order[0])
        for exi, ex in enumerate(ex_order):
            for dcp in range(8):
                k = ga[0] % 2
                ga[0] += 1
                for j in range(2):
                    dc = 2 * dcp + j
                    for t in range(8):
                        S.op("pe", lambda e, k=k, j=j, dc=dc, t=t: e.matmul(banks[k][:, j * 256:(j + 1) * 256],
                                                                            lhsT=x2nb[:, t, dc * 128:(dc + 1) * 128], rhs=Sel[:, t, :],
                                                                            start=(t == 0), stop=(t == 7), skip_group_check=True),
                             reads=[X2N[t], SEL], writes=[PB[k]])
                evac_copy(xg[:, 2 * dcp:2 * dcp + 2, :], banks[k][:].rearrange("p (j s) -> p j s", j=2), [PB[k]], [XG])
            for sb in range(2):
                for tq in range(2):
                    k = ga[0] % 2
                    ga[0] += 1
                    pv = bank_bf(k)[:, 0:512].rearrange("p (j t) -> p j t", j=4)
                    for j in range(4):
                        t = 4 * tq + j
                        S.op("pe", lambda e, pv=pv, j=j, t=t, sb=sb: e.transpose(out=pv[:, j, :], in_=Sel[:, t, sb * 128:(sb + 1) * 128],
                                                                                identity=identB),
                             reads=[SEL, CB], writes=[PB[k]])
                    evac_copy(SelT[:, sb, tq * 512:(tq + 1) * 512], bank_bf(k)[:, 0:512], [PB[k]], [SELT])
            k = ga[0] % 2
            ga[0] += 1
            for sb in range(2):
                for t in range(8):
                    S.op("pe", lambda e, k=k, sb=sb, t=t, ex=ex: e.matmul(banks[k][:, sb * 2:sb * 2 + 2], lhsT=Sel[:, t, sb * 128:(sb + 1) * 128],
                                                                          rhs=gHL[:, t, ex, :], start=(t == 0), stop=(t == 7),
                                                                          skip_group_check=True),
                         reads=[SEL, ROUT[t]], writes=[PB[k]])
            S.op("dve", lambda e, k=k: e.tensor_reduce(out=gsl2[exi % 2], in_=banks[k][:, 0:4].rearrange("p (b two) -> p b two", two=2),
                                                       axis=mybir.AxisListType.X, op=ALU.add), reads=[PB[k]], writes=[GSLB[exi % 2]])
            if exi + 1 < len(ex_order):
                build_sel(ex_order[exi + 1])
            for pc in range(8):
                pi = pidx[0]
                pidx[0] += 1
                s = pi % NRING
                wv = ring[s].rearrange("p (c n) -> p c n", c=NDC)
                for fcj in range(2):
                    fc = 2 * pc + fcj
                    k = 2 + (gu[0] % 2)
                    gu[0] += 1
                    for half in range(2):
                        for dc in range(NDC):
                            S.op("pe", lambda e, k=k, half=half, dc=dc, fcj=fcj, wv=wv: e.matmul(
                                banks[k][:, half * 256:(half + 1) * 256],
                                lhsT=wv[:, dc, half * 256 + fcj * 128:half * 256 + (fcj + 1) * 128], rhs=xg[:, dc, :],
                                start=(dc == 0), stop=(dc == NDC - 1), skip_group_check=True),
                                reads=[RING[s], XG], writes=[PB[k]])
                    i = ewi[0] % 2
                    ewi[0] += 1
                    bg = bgu[:, ex * 32 + fc:ex * 32 + fc + 1]
                    bu = bgu[:, ex * 32 + 16 + fc:ex * 32 + 16 + fc + 1]
                    S.op("dve", lambda e, k=k, i=i, bg=bg: e.tensor_scalar(out=gc_t[i], in0=banks[k][:, 0:256], scalar1=bg, scalar2=7.0,
                                                                           op0=ALU.add, op1=ALU.min), reads=[PB[k], BGU], writes=[EW[i]])
                    S.op("act", lambda e, i=i: e.activation(out=sg_t[i], in_=gc_t[i], func=AF.Sigmoid, scale=1.702),
                         reads=[EW[i]], writes=[EW[i]])
                    S.op("dve", lambda e, k=k, i=i, bu=bu: e.tensor_scalar(out=u1_t[i], in0=banks[k][:, 256:512], scalar1=bu, scalar2=8.0,
                                                                           op0=ALU.add, op1=ALU.min), reads=[PB[k], BGU], writes=[EW[i]])
                    S.op("dve", lambda e, i=i: e.tensor_tensor(out=sg_t[i], in0=sg_t[i], in1=gc_t[i], op=ALU.mult),
                         reads=[EW[i]], writes=[EW[i]])
                    S.op("dve", lambda e, i=i, fc=fc: e.scalar_tensor_tensor(out=actT[:, fc, :], in0=u1_t[i], scalar=-6.0, in1=sg_t[i],
                                                                             op0=ALU.max, op1=ALU.mult), reads=[EW[i]], writes=[ACTT])
                if pi + NRING < len(pieces):
                    issue_piece(pi + NRING)
            def emit_down(dgp):
                pi = pidx[0]
                pidx[0] += 1
                s_ = pi % NRING
                wv = ring[s_].rearrange("p (c n) -> p c n", c=16)
                for sb in range(2):
                    k = 4 + (dnb[0] % 2)
                    dnb[0] += 1
                    for fc in range(16):
                        S.op("pe", lambda e, k=k, fc=fc, sb=sb, wv=wv: e.matmul(banks[k][:], lhsT=actT[:, fc, sb * 128:(sb + 1) * 128],
                                                                                rhs=wv[:, fc, :], start=(fc == 0), stop=(fc == 15)),
                             reads=[ACTT, RING[s_]], writes=[PB[k]])
                    S.op("act", lambda e, k=k, sb=sb, dgp=dgp: e.activation(out=ysl[:, sb, dgp * 512:(dgp + 1) * 512], in_=banks[k][:],
                                                                            func=AF.Copy, scale=gsl2[exi % 2][:, sb:sb + 1]),
                         reads=[PB[k], GSLB[exi % 2]], writes=[YSL[dgp]])
                if pi + NRING < len(pieces):
                    issue_piece(pi + NRING)

            def emit_scatter(dgp):
                for t in range(8):
                    k = 6 + (scb2[0] % 2)
                    scb2[0] += 1
                    for sb in range(2):
                        S.op("pe", lambda e, k=k, sb=sb, t=t, dgp=dgp: e.matmul(banks[k][:], lhsT=SelT[:, sb, t * 128:(t + 1) * 128],
                                                                                rhs=ysl[:, sb, dgp * 512:(dgp + 1) * 512],
                                                                                start=(sb == 0), stop=(sb == 1)),
                             reads=[SELT, YSL[dgp]], writes=[PB[k]])
                    S.op("dve", lambda e, k=k, t=t, dgp=dgp: e.tensor_tensor(out=x2[:, t, dgp * 512:(dgp + 1) * 512], in0=banks[k][:],
                                                                             in1=x2[:, t, dgp * 512:(dgp + 1) * 512], op=ALU.add),
                         reads=[PB[k], X2[t]], writes=[X2[t]])

            emit_down(0)
            for dgp in range(4):
                if dgp + 1 < 4:
                    emit_down(dgp + 1)
                emit_scatter(dgp)

        if debug in ("moe1", "x3"):
            dump([(x2.rearrange("p c d -> p (c d)"), 0, 16384)], X2)
            return nc

        dgf = S.dsem()
        S.alias(EW + YSL + [XG, ACTT, SEL, SELT], GBC)
        S.dma("sync", gbc, gfin_d, dgf, writes=[GBC])
        ot = [V(128 + 8 * i, 8192) for i in range(2)]
        OT = [Buf("ot0"), Buf("ot1")]
        for b_ in OT:
            S.alias(RING + [X2NF, XTC, WRT, GTB, BDP] + ROUT + [RTMP], b_)
        dout = [S.dsem(), S.dsem()]
        ssq3 = small_t[:, 48:56]
        rs3 = small_t[:, 56:64]
        evs = []
        for t in range(8):
            i = t % 2
            S.op("act", lambda e, t=t, i=i: e.activation(out=ot[i], in_=x2[:, t, :], func=AF.Square, accum_out=ssq3[:, t:t + 1]),
                 reads=[X2[t]], writes=[OT[i], STAT])
            S.op("dve", lambda e, t=t: e.tensor_scalar(out=rs3[:, t:t + 1], in0=ssq3[:, t:t + 1], scalar1=1.0 / D, scalar2=EPS,
                                                       op0=ALU.mult, op1=ALU.add), reads=[STAT], writes=[STAT])
            S.op("act", lambda e, t=t: e.activation(out=rs3[:, t:t + 1], in_=rs3[:, t:t + 1], func=AF.Sqrt), reads=[STAT], writes=[STAT])
            S.op("dve", lambda e, t=t: e.reciprocal(out=rs3[:, t:t + 1], in_=rs3[:, t:t + 1]), reads=[STAT], writes=[STAT])
            S.op("dve", lambda e, t=t, i=i: e.scalar_tensor_tensor(out=ot[i], in0=x2[:, t, :], scalar=rs3[:, t:t + 1], in1=gbc,
                                                                   op0=ALU.mult, op1=ALU.mult), reads=[X2[t], STAT, GBC, OT[i]], writes=[OT[i]])
            evs.append(S.dma("sync", out_d[t * 128:(t + 1) * 128, :], ot[i], dout[i], reads=[OT[i]]))
        S.final_wait("sync", evs)
        S.emit()
    return nc


def prep_shared(inp):
    f = lambda a: np.ascontiguousarray(np.asarray(a, dtype=np.float32))
    w_in = f(inp["w_in"])
    sh = {}
    sh["cst"] = _make_cst()
    sh["gmix_bc"] = f(np.broadcast_to(inp["norm_mix"][None, :], (128, D)))
    sh["gffn_bc"] = f(np.broadcast_to(inp["norm_ffn"][None, :], (128, D)))
    sh["gfin_bc"] = f(np.broadcast_to(inp["norm_final"][None, :], (128, D)))
    win3 = w_in.reshape(NDC, 128, 5120)
    watt = np.empty((8, 128, NDC, 384), np.float32)
    wlru = np.empty((8, 128, NDC, 256), np.float32)
    for i in range(8):
        for j, base in enumerate((0, 1024, 2048)):
            watt[i, :, :, j * 128:(j + 1) * 128] = win3[:, :, base + i * 128:base + (i + 1) * 128].transpose(1, 0, 2)
        for j, base in enumerate((3072, 4096)):
            wlru[i, :, :, j * 128:(j + 1) * 128] = win3[:, :, base + i * 128:base + (i + 1) * 128].transpose(1, 0, 2)
    sh["w_att"] = watt
    sh["w_lru"] = wlru
    wabd = np.zeros((8, 128, 256), np.float32)
    wa = f(inp["w_a"])
    wx = f(inp["w_x"])
    for cc in range(8):
        for j in range(2):
            wabd[cc, j * 64:(j + 1) * 64, j * 64:(j + 1) * 64] = wa[2 * cc + j]
            wabd[cc, j * 64:(j + 1) * 64, 128 + j * 64:128 + (j + 1) * 64] = wx[2 * cc + j]
    sh["wabd"] = wabd
    sh["w_out"] = f(inp["w_out"])
    sh["w_router_t"] = f(f(inp["w_router"]).reshape(NDC, 128, NE).transpose(1, 0, 2))
    bgu = f(inp["b_gate_up"]).reshape(NE, 2048, 2)
    bt = np.empty((128, NE, 32), np.float32)
    bt[:, :, 0:16] = bgu[:, :, 0].reshape(NE, 16, 128).transpose(2, 0, 1)
    bt[:, :, 16:32] = bgu[:, :, 1].reshape(NE, 16, 128).transpose(2, 0, 1)
    sh["bgu"] = bt.reshape(128, NE * 32)
    sh["b_down"] = f(inp["b_down"])
    wgu = np.asarray(inp["w_gate_up"], dtype=np.float32).reshape(NE, NDC, 128, 8, 256, 2)
    sh["wgu_t"] = np.ascontiguousarray(wgu.transpose(0, 3, 2, 1, 5, 4)).reshape(NE, 8, 128, 8192)
    wdn = np.asarray(inp["w_down"], dtype=np.float32).reshape(NE, 16, 128, 4, 512)
    sh["wdn_t"] = np.ascontiguousarray(wdn.transpose(0, 3, 2, 1, 4)).reshape(NE, 4, 128, 8192)
    smv = np.zeros((128, NSM), np.float32)
    gm = np.concatenate([f(inp["attn_out_norm"]), f(inp["lru_out_norm"])])
    smv[:, SM_GMIXT:SM_GMIXT + 16] = gm.reshape(16, 128).T
    cw = f(inp["conv_w"])
    smv[:, SM_CONVW:SM_CONVW + 32] = cw.reshape(4, 8, 128).transpose(2, 1, 0).reshape(128, 32)
    smv[:, SM_CONVB:SM_CONVB + 8] = f(inp["conv_b"]).reshape(8, 128).T
    smv[:, SM_BA:SM_BA + 8] = f(inp["b_a"]).reshape(8, 128).T
    smv[:, SM_BX:SM_BX + 8] = f(inp["b_x"]).reshape(8, 128).T
    smv[:, SM_LAM:SM_LAM + 8] = f(inp["lru_lambda"]).reshape(8, 128).T
    smv[:, SM_BROUT:SM_BROUT + NE] = f(inp["b_router"])[None, :]
    sh["smalls"] = smv
    return sh


def core_inputs(inp, sh, c):
    b, half = c // 2, c % 2
    x = np.asarray(inp["x"], dtype=np.float32)
    if half == 1:
        xw = np.ascontiguousarray(x[b])
    else:
        xw = np.concatenate([np.zeros((OWN, D), np.float32), x[b, :OWN]], axis=0)
    m = dict(sh)
    smv = sh["smalls"].copy()
    fl = 1.0 if half == 1 else 0.0
    smv[:, SM_FLAG] = fl
    smv[0:64, SM_FLAG + 1] = fl
    smv[64:128, SM_FLAG + 1] = 1.0
    m["smalls"] = smv
    m["xw"] = xw
    return m


_NC_CACHE = {}


def kernel(**inputs):
    sh = prep_shared(inputs)
    in_maps = [core_inputs(inputs, sh, c) for c in range(8)]
    if "nc" not in _NC_CACHE:
        _NC_CACHE["nc"] = build()
    res = run_bass_kernel_spmd(_NC_CACHE["nc"], in_maps, core_ids=list(range(8)))
    out = np.empty((4, 2048, D), np.float32)
    for c in range(8):
        b, half = c // 2, c % 2
        out[b, half * OWN:(half + 1) * OWN, :] = np.asarray(res.results[c]["out"], dtype=np.float32)
    return out
```
